# Optimizing a Trainium2 kernel written in Bass

```python
import jax, jax.numpy as jnp
from jax import lax
import numpy as np

D_MODEL = 4096
BATCH = 4
SEQ = 4096
DEPTH = 1

CHUNK = 64
LEFT_CHUNKS = 8
BAND = (LEFT_CHUNKS + 1) * CHUNK
LEFT_PAD = LEFT_CHUNKS * CHUNK
POOL_WIDTH = D_MODEL // 4
POOL_WINDOWS = (2, 4, 8, 16)
N_POOL_GROUPS = len(POOL_WINDOWS)
POOL_GROUP_DIM = POOL_WIDTH // N_POOL_GROUPS
ATTN_WIDTH = D_MODEL - POOL_WIDTH
HEAD_DIM = 128
N_HEADS = ATTN_WIDTH // HEAD_DIM
MAX_REL = 128
N_REL = 2 * MAX_REL + 1
IN_WIDTH = POOL_WIDTH + 3 * ATTN_WIDTH
N_GROUPS = 4
EXPERTS_PER_GROUP = 8
N_EXPERTS = N_GROUPS * EXPERTS_PER_GROUP
TOP_K = 2
D_EXPERT = D_MODEL // 4
ROW_BLOCK = 128
EPS = 1e-6
MASK_VALUE = -1e30

kernel_name = "hybrid_pool_chunkattn_hmoe"


def _rmsnorm(x, g):
    xf = x.astype(jnp.float32)
    y = xf * lax.rsqrt(jnp.mean(xf * xf, axis=-1, keepdims=True) + EPS)
    return (y * g.astype(jnp.float32)).astype(x.dtype)


def _pool_mixer(u, group_w, scale):
    B, S, _ = u.shape
    uf = u.astype(jnp.float32)
    csum = jnp.concatenate([jnp.zeros((B, 1, POOL_WIDTH), jnp.float32),
                            jnp.cumsum(uf, axis=1)], axis=1)
    t = jnp.arange(S)
    outs = []
    for gi, w in enumerate(POOL_WINDOWS):
        sl = slice(gi * POOL_GROUP_DIM, (gi + 1) * POOL_GROUP_DIM)
        lo = jnp.maximum(t + 1 - w, 0)
        count = (t + 1 - lo).astype(jnp.float32)[None, :, None]
        outs.append((csum[:, 1:, sl] - csum[:, lo, sl]) / count - uf[..., sl])
    pooled = jnp.stack(outs, axis=2)
    mixed = jnp.einsum('bsgc,gcd->bsgd', pooled, group_w.astype(jnp.float32))
    return (mixed.reshape(B, S, POOL_WIDTH) * scale.astype(jnp.float32)).astype(u.dtype)


def _chunked_attention(q, k, v, q_gain, k_gain, rel_bias):
    B, S, H, Dh = q.shape
    n_chunks = S // CHUNK
    q = _rmsnorm(q, q_gain)
    k = _rmsnorm(k, k_gain)
    pad = ((0, 0), (0, 0), (LEFT_PAD, 0), (0, 0))
    kp = jnp.pad(k.transpose(0, 2, 1, 3), pad)
    vp = jnp.pad(v.transpose(0, 2, 1, 3), pad)
    qc = q.reshape(B, n_chunks, CHUNK, H, Dh).transpose(1, 0, 3, 2, 4)
    qi = jnp.arange(CHUNK)[:, None]
    kj = jnp.arange(BAND)[None, :]
    rel_idx = jnp.clip(qi + LEFT_PAD - kj, -MAX_REL, MAX_REL) + MAX_REL
    bias = rel_bias[:, rel_idx].astype(jnp.float32)
    scale = HEAD_DIM ** -0.5

    def one_chunk(args):
        c, qb = args
        kb = lax.dynamic_slice_in_dim(kp, c * CHUNK, BAND, axis=2)
        vb = lax.dynamic_slice_in_dim(vp, c * CHUNK, BAND, axis=2)
        s = jnp.einsum('bhqd,bhkd->bhqk', qb, kb,
                       preferred_element_type=jnp.float32) * scale + bias
        valid = kj >= LEFT_PAD - c * CHUNK
        s = jnp.where(valid[None, None], s, MASK_VALUE)
        p = jax.nn.softmax(s, axis=-1)
        return jnp.einsum('bhqk,bhkd->bhqd', p.astype(vb.dtype), vb)

    o = lax.map(one_chunk, (jnp.arange(n_chunks), qc))
    return o.transpose(1, 0, 3, 2, 4).reshape(B, S, H * Dh)


def _hier_moe(h, w_rg, b_rg, w_re, b_re, w_gate, w_up, w_down):
    B, S, D = h.shape
    N = B * S
    xf = h.reshape(N, D)
    lg = jnp.matmul(xf, w_rg, preferred_element_type=jnp.float32) + b_rg.astype(jnp.float32)
    pg = jax.nn.softmax(lg, axis=-1)
    g_idx = jnp.argmax(lg, axis=-1)
    p_sel = jnp.take_along_axis(pg, g_idx[:, None], axis=1)
    le_all = jnp.einsum('nd,gde->nge', xf, w_re,
                        preferred_element_type=jnp.float32) + b_re.astype(jnp.float32)
    le = jnp.take_along_axis(le_all, g_idx[:, None, None], axis=1)[:, 0]
    top_v, top_i = lax.top_k(le, TOP_K)
    gates = p_sel * jax.nn.softmax(top_v, axis=-1)
    expert = g_idx[:, None].astype(jnp.int32) * EXPERTS_PER_GROUP + top_i.astype(jnp.int32)

    A = N * TOP_K
    e_flat = expert.reshape(A)
    g_flat = gates.reshape(A)
    tok_flat = jnp.repeat(jnp.arange(N, dtype=jnp.int32), TOP_K)
    order = jnp.argsort(e_flat, stable=True)
    e_sorted = e_flat[order]
    counts = jnp.bincount(e_flat, length=N_EXPERTS)
    start = jnp.cumsum(counts) - counts
    pcounts = ((counts + ROW_BLOCK - 1) // ROW_BLOCK) * ROW_BLOCK
    pend = jnp.cumsum(pcounts)
    pstart = pend - pcounts
    dest = pstart[e_sorted] + (jnp.arange(A, dtype=jnp.int32) - start[e_sorted])
    P = A + N_EXPERTS * ROW_BLOCK
    NB = P // ROW_BLOCK
    buf_tok = jnp.full((P,), N, jnp.int32).at[dest].set(tok_flat[order])
    buf_gate = jnp.zeros((P,), jnp.float32).at[dest].set(g_flat[order])
    block_start = jnp.arange(NB, dtype=jnp.int32) * ROW_BLOCK
    block_e = jnp.minimum(jnp.sum(block_start[:, None] >= pend[None, :], axis=1), N_EXPERTS - 1)
    x_pad = jnp.concatenate([xf, jnp.zeros((1, D), xf.dtype)], axis=0)

    def run_block(args):
        e, tb, gb = args
        xb = x_pad[tb]
        a = jax.nn.silu(xb @ w_gate[e]) * (xb @ w_up[e])
        return (a @ w_down[e]) * gb[:, None].astype(xb.dtype)

    ys = lax.map(run_block, (block_e, buf_tok.reshape(NB, ROW_BLOCK),
                             buf_gate.reshape(NB, ROW_BLOCK)))
    out = jnp.zeros((N + 1, D), jnp.float32).at[buf_tok].add(
        ys.reshape(P, D).astype(jnp.float32))[:N]
    return out.astype(h.dtype).reshape(B, S, D)


def setup_inputs(seed: int = 0) -> dict:
    key = jax.random.key(seed)
    ks = jax.random.split(key, 18)
    f32 = jnp.float32
    nrm = lambda k, shape, s: jax.random.normal(k, shape, f32) * s
    return {
        "x": nrm(ks[0], (BATCH, SEQ, D_MODEL), 1.0),
        "norm1_gain": 1.0 + nrm(ks[1], (DEPTH, D_MODEL), 0.02),
        "w_in": nrm(ks[2], (DEPTH, D_MODEL, IN_WIDTH), D_MODEL ** -0.5),
        "pool_group_w": nrm(ks[3], (DEPTH, N_POOL_GROUPS, POOL_GROUP_DIM, POOL_GROUP_DIM),
                            POOL_GROUP_DIM ** -0.5),
        "pool_scale": 1.0 + nrm(ks[4], (DEPTH, POOL_WIDTH), 0.02),
        "q_norm_gain": 1.0 + nrm(ks[5], (DEPTH, HEAD_DIM), 0.02),
        "k_norm_gain": 1.0 + nrm(ks[6], (DEPTH, HEAD_DIM), 0.02),
        "rel_bias": nrm(ks[7], (DEPTH, N_HEADS, N_REL), 0.5),
        "w_out": nrm(ks[8], (DEPTH, D_MODEL, D_MODEL), D_MODEL ** -0.5),
        "norm2_gain": 1.0 + nrm(ks[9], (DEPTH, D_MODEL), 0.02),
        "w_router_group": nrm(ks[10], (DEPTH, D_MODEL, N_GROUPS), D_MODEL ** -0.5),
        "b_router_group": nrm(ks[11], (DEPTH, N_GROUPS), 0.01),
        "w_router_expert": nrm(ks[12], (DEPTH, N_GROUPS, D_MODEL, EXPERTS_PER_GROUP),
                               D_MODEL ** -0.5),
        "b_router_expert": nrm(ks[13], (DEPTH, N_GROUPS, EXPERTS_PER_GROUP), 0.01),
        "w_expert_gate": nrm(ks[14], (DEPTH, N_EXPERTS, D_MODEL, D_EXPERT), D_MODEL ** -0.5),
        "w_expert_up": nrm(ks[15], (DEPTH, N_EXPERTS, D_MODEL, D_EXPERT), D_MODEL ** -0.5),
        "w_expert_down": nrm(ks[16], (DEPTH, N_EXPERTS, D_EXPERT, D_MODEL), D_EXPERT ** -0.5),
    }


def reference(x, norm1_gain, w_in, pool_group_w, pool_scale, q_norm_gain, k_norm_gain,
              rel_bias, w_out, norm2_gain, w_router_group, b_router_group, w_router_expert,
              b_router_expert, w_expert_gate, w_expert_up, w_expert_down):
    B, S, _ = x.shape
    for l in range(DEPTH):
        h = _rmsnorm(x, norm1_gain[l])
        proj = h @ w_in[l]
        u = proj[..., :POOL_WIDTH]
        q = proj[..., POOL_WIDTH:POOL_WIDTH + ATTN_WIDTH].reshape(B, S, N_HEADS, HEAD_DIM)
        k = proj[..., POOL_WIDTH + ATTN_WIDTH:POOL_WIDTH + 2 * ATTN_WIDTH].reshape(
            B, S, N_HEADS, HEAD_DIM)
        v = proj[..., POOL_WIDTH + 2 * ATTN_WIDTH:].reshape(B, S, N_HEADS, HEAD_DIM)
        y_pool = _pool_mixer(u, pool_group_w[l], pool_scale[l])
        y_attn = _chunked_attention(q, k, v, q_norm_gain[l], k_norm_gain[l], rel_bias[l])
        x = x + jnp.concatenate([y_pool, y_attn], axis=-1) @ w_out[l]
        h2 = _rmsnorm(x, norm2_gain[l])
        x = x + _hier_moe(h2, w_router_group[l], b_router_group[l], w_router_expert[l],
                          b_router_expert[l], w_expert_gate[l], w_expert_up[l],
                          w_expert_down[l])
    return x
```

```python
import numpy as np
from contextlib import ExitStack
import concourse.bass as bass
import concourse.mybir as mybir
from concourse.bass_utils import run_bass_kernel_spmd

F32 = mybir.dt.float32
BF16 = mybir.dt.bfloat16
I32 = mybir.dt.int32
ALU = mybir.AluOpType
AF = mybir.ActivationFunctionType
AX = mybir.AxisListType
_DT_SIZE = {F32: 4, BF16: 2, I32: 4}

D = 4096
KC = 32
T_OWN = 2048
HALO = 512
T_EXT = T_OWN + HALO
NH = 24
CAP = 256
NEXP = 32
ZROW = NEXP * CAP
EPS = 1e-6
NEG = -30000.0


class Arena:
    def __init__(self, handle, nbytes, elem_bytes):
        self.h = handle
        self.nbytes = nbytes
        self.eb = elem_bytes
        self.top = 0
        self.marks = []

    def alloc(self, shape, dtype, align=64):
        sz = _DT_SIZE[dtype]
        n = 1
        for s in shape:
            n *= s
        nb = n * sz
        off = (self.top + align - 1) // align * align
        assert off + nb <= self.nbytes, f"arena overflow {off}+{nb}>{self.nbytes}"
        self.top = off + nb
        ap = self.h[:, off // self.eb:(off + nb) // self.eb]
        if dtype != self.h.dtype:
            ap = ap.bitcast(dtype)
        if len(shape) == 2:
            ap = ap.rearrange("p (a b) -> p a b", b=shape[1])
        elif len(shape) == 3:
            ap = ap.rearrange("p (a b c) -> p a b c", b=shape[1], c=shape[2])
        return ap

    def mark(self):
        self.marks.append(self.top)

    def release(self):
        self.top = self.marks.pop()


class Op:
    __slots__ = ("eng", "fn", "deps", "dma_key", "signal", "event")

    def __init__(self, eng, fn, dma_key):
        self.eng = eng
        self.fn = fn
        self.deps = set()
        self.dma_key = dma_key
        self.signal = dma_key is not None
        self.event = None


class Prog:
    def __init__(self, nc):
        self.nc = nc
        self.ops = []
        self.res = {}
        self.last_on = {}

    def add(self, eng, fn, reads=(), writes=(), accs=(), dma_key=None):
        idx = len(self.ops)
        op = Op(eng, fn, dma_key)
        deps = op.deps
        sk = ("dma", dma_key) if dma_key is not None else ("eng", eng)
        res = self.res
        for r in reads:
            st = res.get(r)
            if st is None:
                st = res[r] = {"pw": None, "aw": {}, "r": {}}
            if st["pw"] is not None:
                deps.add(st["pw"])
            deps.update(st["aw"].values())
        for w in writes:
            st = res.get(w)
            if st is None:
                st = res[w] = {"pw": None, "aw": {}, "r": {}}
            if st["pw"] is not None:
                deps.add(st["pw"])
            deps.update(st["aw"].values())
            deps.update(st["r"].values())
        for a in accs:
            st = res.get(a)
            if st is None:
                st = res[a] = {"pw": None, "aw": {}, "r": {}}
            if st["pw"] is not None:
                deps.add(st["pw"])
            deps.update(st["r"].values())
        for r in reads:
            res[r]["r"][sk] = idx
        for w in writes:
            st = res[w]
            st["pw"] = idx
            st["aw"] = {}
            st["r"] = {}
        for a in accs:
            res[a]["aw"][sk] = idx
        deps.discard(idx)
        self.ops.append(op)
        self.last_on[sk] = idx
        return idx

    def barrier(self):
        lasts = set(self.last_on.values())
        for eng in ("sp", "pe", "act", "dve", "pool"):
            op = Op(eng, None, None)
            op.deps = set(lasts)
            self.ops.append(op)
        self.res = {}

    def emit(self):
        nc = self.nc
        ops = self.ops
        for op in ops:
            nd = set()
            for d in op.deps:
                dop = ops[d]
                if dop.dma_key is None and op.dma_key is None and dop.eng == op.eng and op.eng == "pe":
                    continue
                nd.add(d)
            op.deps = nd
            for d in nd:
                ops[d].signal = True
        cnt = {}
        for op in ops:
            if op.dma_key is not None:
                k = ("dma", op.dma_key)
                cnt[k] = cnt.get(k, 0) + 16
                op.event = (k, cnt[k])
            elif op.signal:
                k = ("eng", op.eng)
                cnt[k] = cnt.get(k, 0) + 1
                op.event = (k, cnt[k])
        sem_keys = list(cnt.keys())
        self.n_sems = len(sem_keys)
        with ExitStack() as es:
            sems = {}
            for i, k in enumerate(sem_keys):
                sems[k] = es.enter_context(nc.semaphore(f"s{i}"))
            block = es.enter_context(nc.Block())
            engmap = {"sp": "sync", "pe": "tensor", "act": "scalar", "dve": "vector", "pool": "gpsimd"}

            def make(engname):
                def body(e):
                    seen = {}
                    for op in ops:
                        if op.eng != engname:
                            continue
                        for d in sorted(op.deps):
                            k, v = ops[d].event
                            if seen.get(k, 0) < v:
                                e.wait_ge(sems[k], v)
                                seen[k] = v
                        if op.fn is None:
                            continue
                        ins = op.fn(e)
                        if op.dma_key is not None:
                            ins.then_inc(sems[op.event[0]], 16)
                        elif op.signal:
                            ins.then_inc(sems[op.event[0]], 1)
                return body

            for engname, attr in engmap.items():
                getattr(block, attr)(make(engname))


def build_program(phases="ABCDEFG", dbg=False):
    nc = bass.Bass("TRN2", target_bir_lowering=False)

    def din(name, shape, dt=F32):
        return nc.dram_tensor(name, shape, dt, kind="ExternalInput").ap()

    skind = "ExternalOutput" if dbg else "Internal"

    def dscr(name, shape, dt):
        return nc.dram_tensor(name, shape, dt, kind=skind).ap()

    xh = din("xh", [T_EXT, D])
    w_in = din("w_in", [D, 10240])
    w_out = din("w_out", [D, D])
    g1_bc = din("g1_bc", [128, D])
    g2_bc = din("g2_bc", [128, D])
    qg_col = din("qg_col", [128, 1])
    kg_col = din("kg_col", [128, 1])
    pscale = din("pscale", [128, 8])
    gw_in = din("gw", [128, 4 * 2 * 256])
    invc_in = din("invc", [128, 4 * T_OWN])
    biasT = din("biasT", [NH, 128, 640])
    maskc_in = din("maskc", [128, 640])
    halo_in = din("halo_col", [128, 1])
    wr_in = din("wr", [128, KC * 36])
    br_in = din("br_bc", [128, 36])
    ebase_in = din("ebase_bc", [128, 32])
    ident_in = din("ident", [128, 128])
    ones_in = din("ones", [128, 128])
    ltri_in = din("ltri", [128, 128])
    use_moe = "F" in phases
    if use_moe:
        wg = din("wg", [NEXP * D, 1024])
        wu = din("wu", [NEXP * D, 1024])
        wd = din("wd", [NEXP * 1024, D])
    out = nc.dram_tensor("out", [T_OWN, D], F32, kind="ExternalOutput").ap()

    qT_s = dscr("qT_s", [NH * 128, T_OWN], BF16)
    kT_s = dscr("kT_s", [NH * 128, T_EXT], BF16)
    v_s = dscr("v_s", [T_EXT, 3072], BF16)
    uT_s = dscr("uT_s", [8 * 128, 2176], F32)
    x1_s = dscr("x1_s", [T_OWN, D], F32)
    Hs = dscr("Hs", [NEXP * CAP, D], BF16)
    Ys = dscr("Ys", [NEXP * CAP + 128, D], F32)
    rt_dbg = dscr("rt_dbg", [128, 16 * 4], F32)

    SB_BYTES = 212000
    sb_h = nc.alloc_sbuf_tensor("arena", [128, SB_BYTES // 2], BF16)
    ps_h = nc.alloc_psum_tensor("psarena", [128, 4096], F32)
    A = Arena(sb_h, SB_BYTES, 2)
    P = Prog(nc)

    def bank(b, n=512, dt=F32):
        ap = ps_h[:, b * 512:(b + 1) * 512]
        if dt == BF16:
            return ap.bitcast(BF16)[:, 0:n]
        return ap[:, 0:n]

    ident_bf = A.alloc([128], BF16)
    ident_f = A.alloc([128], F32)
    ones_bf = A.alloc([128], BF16)
    ltri_bf = A.alloc([128], BF16)
    qg = A.alloc([2], F32)
    kg = A.alloc([2], F32)
    halo = A.alloc([2], F32)
    slot_i = A.alloc([16, 2], I32)
    gidx_i = A.alloc([16, 2], I32)
    gate_all = A.alloc([16, 2], F32)

    P.add("pool", lambda e: e.dma_start(out=ident_bf, in_=ident_in), writes=["ident_bf"], dma_key="c0")
    P.add("pool", lambda e: e.dma_start(out=ones_bf, in_=ones_in), writes=["ones_bf"], dma_key="c1")
    P.add("pool", lambda e: e.dma_start(out=ltri_bf, in_=ltri_in), writes=["ltri_bf"], dma_key="c2")
    P.add("sp", lambda e: e.dma_start(out=ident_f, in_=ident_in), writes=["ident_f"], dma_key="c3")
    P.add("sp", lambda e: e.dma_start(out=qg[:, 0:1], in_=qg_col), writes=["qg0"], dma_key="c4")
    P.add("sp", lambda e: e.dma_start(out=kg[:, 0:1], in_=kg_col), writes=["kg0"], dma_key="c5")
    P.add("sp", lambda e: e.dma_start(out=halo[:, 0:1], in_=halo_in), writes=["halo"], dma_key="c6")
    P.add("dve", lambda e: e.tensor_scalar(out=qg[:, 1:2], in0=qg[:, 0:1], scalar1=float(128 ** -0.5), scalar2=None,
                                           op0=ALU.mult), reads=["qg0"], writes=["qg"])
    P.add("dve", lambda e: e.tensor_copy(out=kg[:, 1:2], in_=kg[:, 0:1]), reads=["kg0"], writes=["kg"])
    if "A" in phases:
        A.mark()
        hT = A.alloc([KC, 1280], BF16)
        gbc = A.alloc([D], F32)
        G = 512
        NS = 2
        wb = [A.alloc([KC, G], BF16), None]
        w_in_v = w_in.rearrange("(kc p) n -> p kc n", p=128)

        def load_w(gi):
            s = gi % NS
            P.add("pool", lambda e, gi=gi, s=s: e.dma_start(out=wb[s], in_=w_in_v[:, :, gi * G:(gi + 1) * G]),
                  writes=[("wb", s)], dma_key=("wb", s))

        P.add("sp", lambda e: e.dma_start(out=gbc, in_=g1_bc), writes=["gbc"], dma_key="gbc")
        for hh in range(2):
            load_w(0)
            A.mark()
            xs = [A.alloc([D], F32) for _ in range(2)]
            xn = [A.alloc([D], BF16) for _ in range(2)]
            junkA = A.alloc([D], BF16)
            st = [A.alloc([4], F32) for _ in range(2)]
            for i in range(10):
                ti = hh * 10 + i
                s = i % 2
                P.add("sp", lambda e, ti=ti, s=s: e.dma_start(out=xs[s], in_=xh[ti * 128:(ti + 1) * 128, :]),
                      writes=[("xs", s)], dma_key=("xs", s))
                P.add("act", lambda e, s=s, junkA=junkA: e.activation(out=junkA, in_=xs[s], func=AF.Square,
                                                                       accum_out=st[s][:, 0:1]),
                      reads=[("xs", s)], writes=["junkA", ("st0", s)])
                P.add("act", lambda e, s=s: e.activation(out=st[s][:, 1:2], in_=st[s][:, 0:1], func=AF.Sqrt,
                                                         scale=1.0 / D, bias=EPS),
                      reads=[("st0", s)], writes=[("st1", s)])
                P.add("dve", lambda e, s=s: e.reciprocal(out=st[s][:, 2:3], in_=st[s][:, 1:2]),
                      reads=[("st1", s)], writes=[("st2", s)])
                P.add("dve", lambda e, s=s: e.scalar_tensor_tensor(out=xn[s], in0=xs[s], scalar=st[s][:, 2:3], in1=gbc,
                                                                   op0=ALU.mult, op1=ALU.mult),
                      reads=[("xs", s), ("st2", s), "gbc"], writes=[("xn", s)])
                for c in range(KC):
                    pb = 6 + (c // 4) % 2
                    P.add("pe", lambda e, c=c, s=s, pb=pb: e.transpose(
                        out=bank(pb, 512, BF16)[:, (c % 4) * 128:(c % 4 + 1) * 128],
                        in_=xn[s][:, c * 128:(c + 1) * 128], identity=ident_bf),
                        reads=[("xn", s), "ident_bf"], writes=[("ps", pb)])
                    if c % 4 == 3:
                        c0 = c - 3
                        src = bank(pb, 512, BF16).rearrange("p (a b) -> p a b", b=128)
                        if (c // 4) % 2:
                            P.add("dve", lambda e, c0=c0, i=i, src=src: e.tensor_copy(
                                out=hT[:, c0:c0 + 4, i * 128:(i + 1) * 128], in_=src),
                                reads=[("ps", pb)], accs=[("hT", i)])
                        else:
                            P.add("act", lambda e, c0=c0, i=i, src=src: e.copy(
                                out=hT[:, c0:c0 + 4, i * 128:(i + 1) * 128], in_=src),
                                reads=[("ps", pb)], accs=[("hT", i)])
            P.barrier()
            A.release()
            A.mark()
            wb[1] = A.alloc([KC, G], BF16)
            sq = [A.alloc([512], BF16) for _ in range(3)]
            rt = [A.alloc([512], F32) for _ in range(3)]
            qo = [A.alloc([512], BF16) for _ in range(3)]
            uo = [A.alloc([512], F32) for _ in range(2)]
            vo = [A.alloc([512], BF16) for _ in range(2)]
            ngroups = 20
            blk = [0]
            pending = []

            def flush():
                while pending:
                    pending.pop(0)()

            for gi in range(ngroups):
                if gi + 1 < ngroups:
                    load_w(gi + 1)
                s = gi % NS
                wkey = ("wb", s)
                if gi >= 14:
                    flush()
                    vc0 = (gi - 14) * 512
                    for i in range(10):
                        b = blk[0] % 3
                        blk[0] += 1
                        for kc in range(KC):
                            P.add("pe", lambda e, kc=kc, i=i, b=b, s=s: e.matmul(
                                bank(b), lhsT=hT[:, kc, i * 128:(i + 1) * 128], rhs=wb[s][:, kc, :],
                                start=(kc == 0), stop=(kc == KC - 1)),
                                reads=[wkey, ("hT", i)], writes=[("ps", b)])
                        vs_ = i % 2
                        ti = hh * 10 + i
                        if i % 2:
                            P.add("dve", lambda e, b=b, vs_=vs_: e.tensor_copy(out=vo[vs_], in_=bank(b)),
                                  reads=[("ps", b)], writes=[("vo", vs_)])
                        else:
                            P.add("act", lambda e, b=b, vs_=vs_: e.copy(out=vo[vs_], in_=bank(b)),
                                  reads=[("ps", b)], writes=[("vo", vs_)])
                        P.add("sp", lambda e, vs_=vs_, ti=ti, vc0=vc0: e.dma_start(
                            out=v_s[ti * 128:(ti + 1) * 128, vc0:vc0 + 512], in_=vo[vs_]),
                            reads=[("vo", vs_)], accs=["v_s"], dma_key=("vo", vs_))
                    continue
                if gi < 2:
                    kind = "u"
                    lo = 384 if hh == 0 else 0
                elif gi < 8:
                    kind = "q"
                    lo = 512 if hh == 0 else 0
                else:
                    kind = "k"
                    lo = 0
                nblocks = []
                n0 = lo
                while n0 < 1280:
                    nsz = min(512, 1280 - n0)
                    nblocks.append((n0, nsz))
                    n0 += nsz
                for ct in range(4):
                    for (n0, nsz) in nblocks:
                        b = blk[0] % 3
                        blk[0] += 1
                        tiles = list(range(n0 // 128, (n0 + nsz + 127) // 128))
                        for kc in range(KC):
                            P.add("pe", lambda e, kc=kc, ct=ct, n0=n0, nsz=nsz, b=b, s=s: e.matmul(
                                bank(b, nsz), lhsT=wb[s][:, kc, ct * 128:(ct + 1) * 128], rhs=hT[:, kc, n0:n0 + nsz],
                                start=(kc == 0), stop=(kc == KC - 1)),
                                reads=[wkey] + [("hT", j) for j in tiles], writes=[("ps", b)])
                        ext0 = hh * 1280 + n0
                        if kind == "u":
                            c = gi * 4 + ct
                            us = blk[0] % 2
                            P.add("act", lambda e, b=b, nsz=nsz, us=us: e.copy(out=uo[us][:, 0:nsz], in_=bank(b, nsz)),
                                  reads=[("ps", b)], writes=[("uo", us)])
                            P.add("sp", lambda e, c=c, us=us, nsz=nsz, ext0=ext0: e.dma_start(
                                out=uT_s[c * 128:(c + 1) * 128, ext0 - 384:ext0 - 384 + nsz], in_=uo[us][:, 0:nsz]),
                                reads=[("uo", us)], accs=["uT_s"], dma_key=("uo", us))
                            continue
                        if kind == "q":
                            h = (gi - 2) * 4 + ct
                            gcol = qg[:, 1:2]
                            gk = "qg"
                            dst = qT_s[h * 128:(h + 1) * 128, ext0 - 512:ext0 - 512 + nsz]
                            dkey = "qT_s"
                        else:
                            h = (gi - 8) * 4 + ct
                            gcol = kg[:, 1:2]
                            gk = "kg"
                            dst = kT_s[h * 128:(h + 1) * 128, ext0:ext0 + nsz]
                            dkey = "kT_s"
                        r = (blk[0] - 1) % 3
                        b2 = 3 + (blk[0] - 1) % 2
                        P.add("act", lambda e, b=b, nsz=nsz, r=r: e.activation(out=sq[r][:, 0:nsz], in_=bank(b, nsz),
                                                                              func=AF.Square),
                              reads=[("ps", b)], writes=[("sq", r)])

                        def stage2(b=b, nsz=nsz, r=r, b2=b2, gcol=gcol, gk=gk, dst=dst, dkey=dkey):
                            P.add("pe", lambda e: e.matmul(bank(b2, nsz), lhsT=ones_bf, rhs=sq[r][:, 0:nsz],
                                                           start=True, stop=True),
                                  reads=["ones_bf", ("sq", r)], writes=[("ps", b2)])
                            P.add("act", lambda e: e.activation(out=rt[r][:, 0:nsz], in_=bank(b2, nsz), func=AF.Sqrt,
                                                                scale=1.0 / 128, bias=EPS),
                                  reads=[("ps", b2)], writes=[("rt", r)])
                            P.add("dve", lambda e: e.reciprocal(out=rt[r][:, 0:nsz], in_=rt[r][:, 0:nsz]),
                                  reads=[("rt", r)], writes=[("rt", r)])
                            P.add("dve", lambda e: e.scalar_tensor_tensor(
                                out=qo[r][:, 0:nsz], in0=bank(b, nsz), scalar=gcol, in1=rt[r][:, 0:nsz],
                                op0=ALU.mult, op1=ALU.mult),
                                reads=[("ps", b), ("rt", r), gk], writes=[("qo", r)])
                            P.add("sp", lambda e: e.dma_start(out=dst, in_=qo[r][:, 0:nsz]),
                                  reads=[("qo", r)], accs=[dkey], dma_key=("qo", r))

                        flush()
                        pending.append(stage2)
            flush()
            P.barrier()
            A.release()
        A.release()

    if "C" in phases:
        A.mark()
        yT = A.alloc([KC, T_OWN], BF16)
        A.mark()
        maskc = A.alloc([640], F32)
        qh = [A.alloc([T_OWN], BF16) for _ in range(2)]
        kh = [A.alloc([T_EXT], BF16) for _ in range(2)]
        vh = [A.alloc([20, 130], BF16) for _ in range(2)]
        bh = [A.alloc([640], F32) for _ in range(2)]
        bhi = [A.alloc([640], BF16) for _ in range(2)]
        blo = [A.alloc([640], BF16) for _ in range(2)]
        Pb = [A.alloc([640], BF16) for _ in range(2)]
        rzc = [A.alloc([2], F32) for _ in range(2)]
        ytm = [A.alloc([128], BF16) for _ in range(2)]
        P.add("sp", lambda e: e.dma_start(out=maskc, in_=maskc_in), writes=["maskc"], dma_key="maskc")
        for s_ in range(2):
            P.add("dve", lambda e, s_=s_: e.memset(vh[s_][:, :, 128:130], 1.0), accs=[("vh", s_)])
        v_v = v_s.rearrange("(t p) c -> p t c", p=128)

        def load_head(h):
            s = h % 2
            P.add("sp", lambda e, h=h, s=s: e.dma_start(out=qh[s], in_=qT_s[h * 128:(h + 1) * 128, :]),
                  reads=["qT_s"], writes=[("qh", s)], dma_key=("qh", s))
            P.add("sp", lambda e, h=h, s=s: e.dma_start(out=kh[s], in_=kT_s[h * 128:(h + 1) * 128, :]),
                  reads=["kT_s"], writes=[("kh", s)], dma_key=("kh", s))
            P.add("sp", lambda e, h=h, s=s: e.dma_start(out=vh[s][:, :, 0:128], in_=v_v[:, :, h * 128:(h + 1) * 128]),
                  reads=["v_s"], accs=[("vh", s)], dma_key=("vh", s))
            P.add("sp", lambda e, h=h, s=s: e.dma_start(out=bh[s], in_=biasT[h]),
                  writes=[("bh0", s)], dma_key=("bh", s))

        def fix_head(h):
            s = h % 2
            P.add("dve", lambda e, s=s: e.tensor_tensor(out=bh[s], in0=bh[s], in1=maskc, op=ALU.add),
                  reads=[("bh0", s), "maskc"], writes=[("bh", s)])
            P.add("dve", lambda e, s=s: e.tensor_copy(out=bhi[s], in_=bh[s]), reads=[("bh", s)], writes=[("bhi", s)])
            P.add("dve", lambda e, s=s: e.tensor_tensor(out=blo[s], in0=bh[s], in1=bhi[s], op=ALU.subtract),
                  reads=[("bh", s), ("bhi", s)], writes=[("blo", s)])

        iters = [(h, t) for h in range(NH) for t in range(16)]
        NI = len(iters)

        def S_of(u):
            return ps_h[:, 2 * u * 512: 2 * u * 512 + 640]

        def rec_qk(it):
            h, t = iters[it]
            s = h % 2
            u = it % 2
            S = S_of(u)
            for kt in range(5):
                Sk = S[:, kt * 128:(kt + 1) * 128]
                P.add("pe", lambda e, kt=kt, t=t, s=s, Sk=Sk: e.matmul(
                    Sk, lhsT=kh[s][:, (t + kt) * 128:(t + kt + 1) * 128],
                    rhs=qh[s][:, t * 128:(t + 1) * 128], start=True, stop=False),
                    reads=[("kh", s), ("qh", s)], writes=[("S", u)])
                P.add("pe", lambda e, kt=kt, s=s, Sk=Sk: e.matmul(
                    Sk, lhsT=ident_bf, rhs=bhi[s][:, kt * 128:(kt + 1) * 128], start=False, stop=False),
                    reads=["ident_bf", ("bhi", s)], writes=[("S", u)])
                P.add("pe", lambda e, kt=kt, s=s, Sk=Sk: e.matmul(
                    Sk, lhsT=ident_bf, rhs=blo[s][:, kt * 128:(kt + 1) * 128], start=False, stop=True),
                    reads=["ident_bf", ("blo", s)], writes=[("S", u)])

        def rec_softmax(it):
            h, t = iters[it]
            u = it % 2
            S = S_of(u)
            nh_ = max(0, 4 - t)
            if nh_ > 0:
                P.add("act", lambda e, u=u, nh_=nh_, S=S: e.activation(out=Pb[u][:, 0:nh_ * 128], in_=S[:, 0:nh_ * 128],
                                                                      func=AF.Exp, bias=halo[:, 0:1]),
                      reads=[("S", u), "halo"], accs=[("Pb", u)])
            P.add("act", lambda e, u=u, nh_=nh_, S=S: e.activation(out=Pb[u][:, nh_ * 128:640], in_=S[:, nh_ * 128:640],
                                                                  func=AF.Exp),
                  reads=[("S", u)], accs=[("Pb", u)])

        def rec_pv(it):
            h, t = iters[it]
            s = h % 2
            u = it % 2
            O = bank(4 + u)[:, 0:129]
            for kt in range(5):
                P.add("pe", lambda e, kt=kt, t=t, s=s, u=u, O=O: e.matmul(
                    O, lhsT=Pb[u][:, kt * 128:(kt + 1) * 128], rhs=vh[s][:, t + kt, 0:129],
                    start=(kt == 0), stop=(kt == 4)),
                    reads=[("vh", s), ("Pb", u)], writes=[("OZ", u)])
            P.add("dve", lambda e, u=u, O=O: e.reciprocal(out=rzc[u][:, 0:1], in_=O[:, 128:129]),
                  reads=[("OZ", u)], writes=[("rzc", u)])
            P.add("dve", lambda e, u=u, O=O: e.tensor_scalar(out=ytm[u], in0=O[:, 0:128], scalar1=rzc[u][:, 0:1],
                                                             scalar2=None, op0=ALU.mult),
                  reads=[("OZ", u), ("rzc", u)], writes=[("ytm", u)])

        def rec_tr(it):
            h, t = iters[it]
            u = it % 2
            tb = 6 + u
            TR = bank(tb, 512, BF16)[:, 0:128]
            P.add("pe", lambda e, u=u, TR=TR: e.transpose(out=TR, in_=ytm[u], identity=ident_bf),
                  reads=[("ytm", u), "ident_bf"], writes=[("ps", tb)])
            P.add("dve", lambda e, TR=TR, h=h, t=t: e.tensor_copy(out=yT[:, 8 + h, t * 128:(t + 1) * 128], in_=TR),
                  reads=[("ps", tb)], accs=["yT"])

        load_head(0)
        fix_head(0)
        rec_qk(0)
        rec_softmax(0)
        rec_qk(1)
        for it in range(NI):
            h, t = iters[it]
            if t == 0 and h + 1 < NH:
                load_head(h + 1)
            if t == 10 and h + 1 < NH:
                fix_head(h + 1)
            if it + 1 < NI:
                rec_softmax(it + 1)
            if it + 2 < NI:
                rec_qk(it + 2)
            rec_pv(it)
            if it >= 1:
                rec_tr(it - 1)
        rec_tr(NI - 1)
        P.barrier()
        A.release()

        A.mark()
        gwb = A.alloc([4, 2, 256], BF16)
        psc = A.alloc([8], F32)
        ut = [A.alloc([2176], F32) for _ in range(2)]
        sa = A.alloc([2176], F32)
        sbuf_b = A.alloc([2176], F32)
        invc = [A.alloc([T_OWN], F32) for _ in range(1)]
        pooled = [A.alloc([2, T_OWN], BF16) for _ in range(1)]
        P.add("pool", lambda e: e.dma_start(out=gwb, in_=gw_in.rearrange("p (g k n) -> p g k n", g=4, k=2)),
              writes=["gwb"], dma_key="gwb")
        P.add("sp", lambda e: e.dma_start(out=psc, in_=pscale), writes=["psc"], dma_key="psc")
        invc_v = invc_in.rearrange("p (g t) -> p g t", g=4)
        ev = 0
        for g in range(4):
            gs = 0
            P.add("sp", lambda e, g=g, gs=gs: e.dma_start(out=invc[gs], in_=invc_v[:, g, :]),
                  writes=[("invc", gs)], dma_key=("invc", gs))
            for kc2 in range(2):
                c = g * 2 + kc2
                us = c % 2
                P.add("sp", lambda e, c=c, us=us: e.dma_start(out=ut[us], in_=uT_s[c * 128:(c + 1) * 128, :]),
                      reads=["uT_s"], writes=[("ut", us)], dma_key=("ut", us))
                cur = ut[us]
                curk = ("ut", us)
                bufs = [(sa, "sa"), (sbuf_b, "sbb")]
                for j in range(g + 1):
                    sh = 1 << j
                    dstb, dk = bufs[j % 2]
                    P.add("dve", lambda e, cur=cur, dstb=dstb, sh=sh: e.tensor_tensor(
                        out=dstb[:, 16:2176], in0=cur[:, 16:2176], in1=cur[:, 16 - sh:2176 - sh], op=ALU.add),
                        reads=[curk], writes=[dk])
                    cur, curk = dstb, dk
                other, ok = bufs[(g + 1) % 2]
                P.add("dve", lambda e, cur=cur, other=other, gs=gs: e.tensor_tensor(
                    out=other[:, 128:2176], in0=cur[:, 128:2176], in1=invc[gs], op=ALU.mult),
                    reads=[curk, ("invc", gs)], writes=[ok])
                P.add("dve", lambda e, other=other, us=us, gs=gs, kc2=kc2: e.tensor_tensor(
                    out=pooled[gs][:, kc2, :], in0=other[:, 128:2176], in1=ut[us][:, 128:2176], op=ALU.subtract),
                    reads=[ok, ("ut", us)], accs=[("pooled", gs)])
            for oc in range(2):
                for nb in range(4):
                    b = ev % 2
                    ev += 1
                    for kc2 in range(2):
                        P.add("pe", lambda e, g=g, kc2=kc2, oc=oc, nb=nb, b=b, gs=gs: e.matmul(
                            bank(b), lhsT=gwb[:, g, kc2, oc * 128:(oc + 1) * 128],
                            rhs=pooled[gs][:, kc2, nb * 512:(nb + 1) * 512], start=(kc2 == 0), stop=(kc2 == 1)),
                            reads=["gwb", ("pooled", gs)], writes=[("ps", b)])
                    cc = g * 2 + oc
                    P.add("act", lambda e, cc=cc, nb=nb, b=b: e.activation(
                        out=yT[:, cc, nb * 512:(nb + 1) * 512], in_=bank(b), func=AF.Copy, scale=psc[:, cc:cc + 1]),
                        reads=[("ps", b), "psc"], accs=["yT"])
        P.barrier()
        A.release()

        A.mark()
        G2 = 512
        wob = [A.alloc([KC, G2], BF16) for _ in range(2)]
        xt = [A.alloc([G2], F32) for _ in range(3)]
        w_out_v = w_out.rearrange("(kc p) n -> p kc n", p=128)

        def load_wo(j):
            s = j % 2
            P.add("pool", lambda e, j=j, s=s: e.dma_start(out=wob[s], in_=w_out_v[:, :, j * G2:(j + 1) * G2]),
                  writes=[("wob", s)], dma_key=("wob", s))

        load_wo(0)
        it = 0
        for j in range(D // G2):
            if j + 1 < D // G2:
                load_wo(j + 1)
            s = j % 2
            for i in range(16):
                b = it % 2
                r = it % 3
                it += 1
                P.add("sp", lambda e, i=i, j=j, r=r: e.dma_start(
                    out=xt[r], in_=xh[(4 + i) * 128:(5 + i) * 128, j * G2:(j + 1) * G2]),
                    writes=[("xt", r)], dma_key=("xt", r))
                for kc in range(KC):
                    P.add("pe", lambda e, kc=kc, i=i, b=b, s=s: e.matmul(
                        bank(b, G2), lhsT=yT[:, kc, i * 128:(i + 1) * 128], rhs=wob[s][:, kc, :],
                        start=(kc == 0), stop=(kc == KC - 1)),
                        reads=["yT", ("wob", s)], writes=[("ps", b)])
                P.add("dve", lambda e, b=b, r=r: e.tensor_tensor(out=xt[r], in0=bank(b, G2), in1=xt[r], op=ALU.add),
                      reads=[("ps", b), ("xt", r)], writes=[("xt", r)])
                P.add("pool", lambda e, i=i, j=j, r=r: e.dma_start(
                    out=x1_s[i * 128:(i + 1) * 128, j * G2:(j + 1) * G2], in_=xt[r]),
                    reads=[("xt", r)], accs=["x1_s"], dma_key=("xo", r))
        P.barrier()
        A.release()
        A.release()

    if "E" in phases:
        A.mark()
        gbc2 = A.alloc([D], F32)
        wr = A.alloc([KC, 36], F32)
        brb = A.alloc([36], F32)
        ebase = A.alloc([32], F32)
        carry = A.alloc([32], F32)
        x1t = [A.alloc([D], F32) for _ in range(2)]
        h2 = [A.alloc([D], F32) for _ in range(2)]
        h2b = [A.alloc([D], BF16) for _ in range(2)]
        h2T = [A.alloc([KC, 128], F32) for _ in range(2)]
        st2 = [A.alloc([4], F32) for _ in range(2)]
        rs = [A.alloc([320], F32) for _ in range(2)]
        Mb = [A.alloc([32], BF16) for _ in range(2)]
        junkE = A.alloc([D], BF16)
        P.add("sp", lambda e: e.dma_start(out=gbc2, in_=g2_bc), writes=["gbc2"], dma_key="gbc2")
        P.add("sp", lambda e: e.dma_start(out=wr, in_=wr_in.rearrange("p (k j) -> p k j", j=36)), writes=["wr"], dma_key="wr")
        P.add("sp", lambda e: e.dma_start(out=brb, in_=br_in), writes=["brb"], dma_key="brb")
        P.add("sp", lambda e: e.dma_start(out=ebase, in_=ebase_in), writes=["ebase"], dma_key="ebase")
        P.add("dve", lambda e: e.memset(carry, 0.0), writes=["carry"])
        if use_moe:
            zt = A.alloc([1024], F32)
            P.add("dve", lambda e: e.memset(zt, 0.0), writes=["zt"])
            for q4 in range(4):
                P.add("sp", lambda e, q4=q4: e.dma_start(out=Ys[ZROW:ZROW + 128, q4 * 1024:(q4 + 1) * 1024], in_=zt),
                      reads=["zt"], accs=["Ys"], dma_key="zf2")

        def e_stage1(i):
            s = i % 2
            P.add("sp", lambda e, i=i, s=s: e.dma_start(out=x1t[s], in_=x1_s[i * 128:(i + 1) * 128, :]),
                  reads=["x1_s"], writes=[("x1t", s)], dma_key=("x1t", s))
            P.add("act", lambda e, s=s: e.activation(out=junkE, in_=x1t[s], func=AF.Square, accum_out=st2[s][:, 0:1]),
                  reads=[("x1t", s)], writes=["junkE", ("s0", s)])
            P.add("act", lambda e, s=s: e.activation(out=st2[s][:, 1:2], in_=st2[s][:, 0:1], func=AF.Sqrt,
                                                     scale=1.0 / D, bias=EPS), reads=[("s0", s)], writes=[("s1", s)])
            P.add("dve", lambda e, s=s: e.reciprocal(out=st2[s][:, 2:3], in_=st2[s][:, 1:2]),
                  reads=[("s1", s)], writes=[("s2", s)])
            P.add("dve", lambda e, s=s: e.scalar_tensor_tensor(out=h2[s], in0=x1t[s], scalar=st2[s][:, 2:3], in1=gbc2,
                                                               op0=ALU.mult, op1=ALU.mult),
                  reads=[("x1t", s), ("s2", s), "gbc2"], writes=[("h2", s)])
            P.add("act", lambda e, s=s: e.copy(out=h2b[s], in_=h2[s]), reads=[("h2", s)], writes=[("h2b", s)])
            for c in range(KC):
                pb = 6 + (c // 4) % 2
                P.add("pe", lambda e, c=c, pb=pb, s=s: e.transpose(out=bank(pb)[:, (c % 4) * 128:(c % 4 + 1) * 128],
                                                                    in_=h2[s][:, c * 128:(c + 1) * 128], identity=ident_f),
                      reads=[("h2", s), "ident_f"], writes=[("ps", pb)])
                if c % 4 == 3:
                    c0 = c - 3
                    src = bank(pb).rearrange("p (a b) -> p a b", b=128)
                    if (c // 4) % 2:
                        P.add("dve", lambda e, c0=c0, src=src, s=s: e.tensor_copy(out=h2T[s][:, c0:c0 + 4, :], in_=src),
                              reads=[("ps", pb)], accs=[("h2T", s)])
                    else:
                        P.add("act", lambda e, c0=c0, src=src, s=s: e.copy(out=h2T[s][:, c0:c0 + 4, :], in_=src),
                              reads=[("ps", pb)], accs=[("h2T", s)])
            lg = bank(s, 36)
            for kc in range(KC):
                P.add("pe", lambda e, kc=kc, lg=lg, s=s: e.matmul(lg, lhsT=h2T[s][:, kc, :], rhs=wr[:, kc, :],
                                                                  start=(kc == 0), stop=(kc == KC - 1)),
                      reads=[("h2T", s), "wr"], writes=[("ps", s)])

        def e_stage2(i):
            s = i % 2
            lg = bank(s, 36)
            R = rs[s]
            L = R[:, 0:36]
            mg = R[:, 36:37]
            ohg = R[:, 40:44]
            nmg = R[:, 44:45]
            eg = R[:, 48:52]
            se = R[:, 52:53]
            psel = R[:, 53:54]
            pen = R[:, 56:60]
            Lem = R[:, 64:96]
            m1 = R[:, 96:97]
            m2 = R[:, 97:98]
            dd = R[:, 98:99]
            e2 = R[:, 99:100]
            t1 = R[:, 100:101]
            g1 = R[:, 101:102]
            g2 = R[:, 102:103]
            oh1 = R[:, 104:136]
            Lem2 = R[:, 136:168]
            oh2 = R[:, 168:200]
            Rk = R[:, 200:232]
            ov = R[:, 232:264]
            sv = R[:, 264:296]
            sl = R[:, 296:298]
            slg = R[:, 298:300]
            rk = ("rs", s)

            def dv(fn, reads=(), writes=()):
                P.add("dve", fn, reads=[rk] + list(reads), writes=[rk] + list(writes))

            dv(lambda e: e.tensor_tensor(out=L, in0=lg, in1=brb, op=ALU.add), reads=[("ps", s), "brb"])
            dv(lambda e: e.reduce_max(out=mg, in_=L[:, 0:4], axis=AX.X))
            dv(lambda e: e.tensor_scalar(out=ohg, in0=L[:, 0:4], scalar1=mg, scalar2=None, op0=ALU.is_equal))
            dv(lambda e: e.tensor_scalar(out=nmg, in0=mg, scalar1=-1.0, scalar2=None, op0=ALU.mult))
            P.add("act", lambda e: e.activation(out=eg, in_=L[:, 0:4], func=AF.Exp, bias=nmg, accum_out=se),
                  reads=[rk], writes=[rk])
            dv(lambda e: e.reciprocal(out=psel, in_=se))
            dv(lambda e: e.tensor_scalar(out=pen, in0=ohg, scalar1=-1.0, scalar2=1e30, op0=ALU.add, op1=ALU.mult))
            for g in range(4):
                dv(lambda e, g=g: e.tensor_scalar(
                    out=Lem[:, g * 8:(g + 1) * 8], in0=L[:, 4 + g * 8:12 + g * 8], scalar1=pen[:, g:g + 1], scalar2=None,
                    op0=ALU.add))
            dv(lambda e: e.reduce_max(out=m1, in_=Lem, axis=AX.X))
            dv(lambda e: e.tensor_scalar(out=oh1, in0=Lem, scalar1=m1, scalar2=None, op0=ALU.is_equal))
            dv(lambda e: e.scalar_tensor_tensor(out=Lem2, in0=oh1, scalar=-1e30, in1=Lem, op0=ALU.mult, op1=ALU.add))
            dv(lambda e: e.reduce_max(out=m2, in_=Lem2, axis=AX.X))
            dv(lambda e: e.tensor_scalar(out=oh2, in0=Lem2, scalar1=m2, scalar2=None, op0=ALU.is_equal))
            dv(lambda e: e.tensor_tensor(out=dd, in0=m2, in1=m1, op=ALU.subtract))
            P.add("act", lambda e: e.activation(out=e2, in_=dd, func=AF.Exp), reads=[rk], writes=[rk])
            dv(lambda e: e.tensor_scalar(out=t1, in0=e2, scalar1=1.0, scalar2=None, op0=ALU.add))
            dv(lambda e: e.reciprocal(out=g1, in_=t1))
            dv(lambda e: e.tensor_tensor(out=gate_all[:, i, 0:1], in0=g1, in1=psel, op=ALU.mult), writes=[("gate_all", i)])
            dv(lambda e: e.tensor_tensor(out=g2, in0=e2, in1=g1, op=ALU.mult))
            dv(lambda e: e.tensor_tensor(out=gate_all[:, i, 1:2], in0=g2, in1=psel, op=ALU.mult), writes=[("gate_all", i)])
            P.add("dve", lambda e: e.tensor_tensor(out=Mb[s], in0=oh1, in1=oh2, op=ALU.add),
                  reads=[rk], writes=[("Mb", s)])
            rkb = bank(2, 64)
            P.add("pe", lambda e: e.matmul(rkb[:, 0:32], lhsT=ltri_bf, rhs=Mb[s], start=True, stop=True),
                  reads=["ltri_bf", ("Mb", s)], writes=[("ps", 2)])
            P.add("pe", lambda e: e.matmul(rkb[:, 32:64], lhsT=ones_bf, rhs=Mb[s], start=True, stop=True),
                  reads=["ones_bf", ("Mb", s)], writes=[("ps", 2)])
            dv(lambda e: e.tensor_tensor(out=Rk, in0=rkb[:, 0:32], in1=carry, op=ALU.add), reads=[("ps", 2), "carry"])
            P.add("dve", lambda e: e.tensor_tensor(out=carry, in0=rkb[:, 32:64], in1=carry, op=ALU.add),
                  reads=[("ps", 2), "carry", rk], writes=["carry"])
            dv(lambda e: e.tensor_scalar(out=ov, in0=Rk, scalar1=float(CAP), scalar2=1e6, op0=ALU.is_ge, op1=ALU.mult))
            dv(lambda e: e.tensor_tensor(out=sv, in0=Rk, in1=ebase, op=ALU.add), reads=["ebase"])
            dv(lambda e: e.tensor_tensor(out=sv, in0=sv, in1=ov, op=ALU.add))
            for k, oh in ((0, oh1), (1, oh2)):
                dv(lambda e, oh=oh: e.tensor_tensor(out=ov, in0=oh, in1=sv, op=ALU.mult))
                dv(lambda e, k=k: e.reduce_sum(out=sl[:, k:k + 1], in_=ov, axis=AX.X))
            dv(lambda e: e.tensor_copy(out=slot_i[:, i, :], in_=sl), writes=[("slot_i", i)])
            dv(lambda e: e.tensor_scalar(out=slg, in0=sl, scalar1=float(ZROW), scalar2=None, op0=ALU.min))
            dv(lambda e: e.tensor_copy(out=gidx_i[:, i, :], in_=slg), writes=[("gidx_i", i)])
            if dbg:
                P.add("sp", lambda e: e.dma_start(out=rt_dbg[:, i * 4:i * 4 + 2], in_=sl),
                      reads=[rk], accs=["rt_dbg"], dma_key=("dbg", s))
                P.add("sp", lambda e: e.dma_start(out=rt_dbg[:, i * 4 + 2:i * 4 + 4], in_=gate_all[:, i, :]),
                      reads=[("gate_all", i)], accs=["rt_dbg"], dma_key=("dbg2", s))
            if use_moe:
                for k in range(2):
                    P.add("pool", lambda e, k=k: e.indirect_dma_start(
                        out=Hs[:, :], out_offset=bass.IndirectOffsetOnAxis(ap=slot_i[:, i, k:k + 1], axis=0),
                        in_=h2b[s], in_offset=None, bounds_check=NEXP * CAP - 1, oob_is_err=False),
                        reads=[("h2b", s), ("slot_i", i)], accs=["Hs"], dma_key=("sc", s, k))

        e_stage1(0)
        for i in range(16):
            if i + 1 < 16:
                e_stage1(i + 1)
            e_stage2(i)
        P.barrier()
        A.release()

    if "F" in phases:
        A.mark()
        GU = 256
        NGU = 4
        NDS = 3
        hsb = [A.alloc([2, D], BF16) for _ in range(2)]
        hTe = [A.alloc([KC, CAP], BF16) for _ in range(2)]
        gub = [A.alloc([KC, GU], BF16) for _ in range(NGU)]
        wdb = [A.alloc([8, 512], BF16) for _ in range(NDS)]
        aT = [A.alloc([8, CAP], BF16) for _ in range(2)]
        sg = [A.alloc([CAP], F32) for _ in range(2)]
        yo = [A.alloc([512], F32) for _ in range(4)]
        wg_v = wg.rearrange("(e kc p) n -> e p kc n", p=128, kc=KC)
        wu_v = wu.rearrange("(e kc p) n -> e p kc n", p=128, kc=KC)
        wd_v = wd.rearrange("(e kc p) n -> e p kc n", p=128, kc=8)
        Hs_v = Hs.rearrange("(e b p) d -> e p b d", p=128, b=2)
        gu_items = [(e_, m2, w) for e_ in range(NEXP) for m2 in range(4) for w in (0, 1)]
        d_items = [(e_, cg) for e_ in range(NEXP) for cg in range(8)]
        gu_next = [0]
        d_next = [0]

        def ensure_gu(upto):
            while gu_next[0] <= min(upto, len(gu_items) - 1):
                k = gu_next[0]
                e_, m2, w = gu_items[k]
                s = k % NGU
                src = (wg_v if w == 0 else wu_v)[e_][:, :, m2 * GU:(m2 + 1) * GU]
                P.add("pool", lambda e, s=s, src=src: e.dma_start(out=gub[s], in_=src),
                      writes=[("gub", s)], dma_key=("gub", s))
                gu_next[0] += 1

        def ensure_d(upto):
            while d_next[0] <= min(upto, len(d_items) - 1):
                k = d_next[0]
                e_, cg = d_items[k]
                s = k % NDS
                src = wd_v[e_][:, :, cg * 512:(cg + 1) * 512]
                P.add("pool", lambda e, s=s, src=src: e.dma_start(out=wdb[s], in_=src),
                      writes=[("wdb", s)], dma_key=("wdb", s))
                d_next[0] += 1

        def load_hs(e_):
            s = e_ % 2
            P.add("sp", lambda e, e_=e_, s=s: e.dma_start(out=hsb[s], in_=Hs_v[e_]),
                  reads=["Hs"], writes=[("hsb", s)], dma_key=("hsb", s))

        load_hs(0)
        gi = 0
        di = 0
        yi = 0
        for e_ in range(NEXP):
            s = e_ % 2
            if e_ + 1 < NEXP:
                load_hs(e_ + 1)
            for sbk in range(2):
                for c in range(KC):
                    pb = 4 + (c // 4) % 2
                    P.add("pe", lambda e, c=c, pb=pb, sbk=sbk, s=s: e.transpose(
                        out=bank(pb, 512, BF16)[:, (c % 4) * 128:(c % 4 + 1) * 128],
                        in_=hsb[s][:, sbk, c * 128:(c + 1) * 128], identity=ident_bf),
                        reads=[("hsb", s), "ident_bf"], writes=[("ps", pb)])
                    if c % 4 == 3:
                        c0 = c - 3
                        src = bank(pb, 512, BF16).rearrange("p (a b) -> p a b", b=128)
                        if (c // 4) % 2:
                            P.add("dve", lambda e, c0=c0, src=src, sbk=sbk, s=s: e.tensor_copy(
                                out=hTe[s][:, c0:c0 + 4, sbk * 128:(sbk + 1) * 128], in_=src),
                                reads=[("ps", pb)], accs=[("hTe", s)])
                        else:
                            P.add("act", lambda e, c0=c0, src=src, sbk=sbk, s=s: e.copy(
                                out=hTe[s][:, c0:c0 + 4, sbk * 128:(sbk + 1) * 128], in_=src),
                                reads=[("ps", pb)], accs=[("hTe", s)])
            for m2 in range(4):
                ensure_gu(gi + NGU - 1)
                sgk = gi % NGU
                suk = (gi + 1) % NGU
                for ct in range(2):
                    m = m2 * 2 + ct
                    pu = m % 2
                    bg = pu * 2
                    bu = pu * 2 + 1
                    for kc in range(KC):
                        P.add("pe", lambda e, kc=kc, ct=ct, bg=bg, sgk=sgk, s=s: e.matmul(
                            bank(bg, CAP), lhsT=gub[sgk][:, kc, ct * 128:(ct + 1) * 128], rhs=hTe[s][:, kc, :],
                            start=(kc == 0), stop=(kc == KC - 1)),
                            reads=[("gub", sgk), ("hTe", s)], writes=[("ps", bg)])
                    for kc in range(KC):
                        P.add("pe", lambda e, kc=kc, ct=ct, bu=bu, suk=suk, s=s: e.matmul(
                            bank(bu, CAP), lhsT=gub[suk][:, kc, ct * 128:(ct + 1) * 128], rhs=hTe[s][:, kc, :],
                            start=(kc == 0), stop=(kc == KC - 1)),
                            reads=[("gub", suk), ("hTe", s)], writes=[("ps", bu)])
                    P.add("act", lambda e, bg=bg, pu=pu: e.activation(out=sg[pu], in_=bank(bg, CAP), func=AF.Silu),
                          reads=[("ps", bg)], writes=[("sg", pu)])
                    P.add("dve", lambda e, bu=bu, pu=pu, m=m, s=s: e.tensor_tensor(
                        out=aT[s][:, m, :], in0=bank(bu, CAP), in1=sg[pu], op=ALU.mult),
                        reads=[("ps", bu), ("sg", pu)], accs=[("aT", s)])
                gi += 2
            for cg in range(8):
                ensure_d(di + NDS - 1)
                sd = di % NDS
                di += 1
                for sbk in range(2):
                    b = 6 + yi % 2
                    ys_ = yi % 4
                    yi += 1
                    for kc in range(8):
                        P.add("pe", lambda e, kc=kc, sbk=sbk, b=b, sd=sd, s=s: e.matmul(
                            bank(b), lhsT=aT[s][:, kc, sbk * 128:(sbk + 1) * 128], rhs=wdb[sd][:, kc, :],
                            start=(kc == 0), stop=(kc == 7)),
                            reads=[("aT", s), ("wdb", sd)], writes=[("ps", b)])
                    if yi % 2:
                        P.add("act", lambda e, b=b, ys_=ys_: e.copy(out=yo[ys_], in_=bank(b)),
                              reads=[("ps", b)], writes=[("yo", ys_)])
                    else:
                        P.add("dve", lambda e, b=b, ys_=ys_: e.tensor_copy(out=yo[ys_], in_=bank(b)),
                              reads=[("ps", b)], writes=[("yo", ys_)])
                    r0 = e_ * CAP + sbk * 128
                    P.add("sp", lambda e, r0=r0, cg=cg, ys_=ys_: e.dma_start(
                        out=Ys[r0:r0 + 128, cg * 512:(cg + 1) * 512], in_=yo[ys_]),
                        reads=[("yo", ys_)], accs=["Ys"], dma_key=("yo", ys_))
        P.barrier()
        A.release()

    if "G" in phases:
        A.mark()
        xg = [A.alloc([D], F32) for _ in range(2)]
        y1 = [A.alloc([D], F32) for _ in range(2)]
        y2 = [A.alloc([D], F32) for _ in range(2)]
        for i in range(16):
            s = i % 2
            P.add("sp", lambda e, i=i, s=s: e.dma_start(out=xg[s], in_=x1_s[i * 128:(i + 1) * 128, :]),
                  reads=["x1_s"], writes=[("xg", s)], dma_key=("xg", s))
            if use_moe:
                for k, yb in ((0, y1), (1, y2)):
                    P.add("pool", lambda e, i=i, k=k, s=s, yb=yb: e.indirect_dma_start(
                        out=yb[s], out_offset=None, in_=Ys[:, :],
                        in_offset=bass.IndirectOffsetOnAxis(ap=gidx_i[:, i, k:k + 1], axis=0)),
                        reads=["Ys", "gidx_i"], writes=[("y", k, s)], dma_key=("yg", k, s))
                    P.add("dve", lambda e, i=i, k=k, s=s, yb=yb: e.scalar_tensor_tensor(
                        out=xg[s], in0=yb[s], scalar=gate_all[:, i, k:k + 1], in1=xg[s], op0=ALU.mult, op1=ALU.add),
                        reads=[("y", k, s), ("xg", s), "gate_all"], writes=[("xg", s)])
            P.add("sp", lambda e, i=i, s=s: e.dma_start(out=out[i * 128:(i + 1) * 128, :], in_=xg[s]),
                  reads=[("xg", s)], accs=["out"], dma_key=("og", s))
        A.release()
    P.barrier()
    P.emit()
    return nc, P


def make_consts():
    ident = np.eye(128, dtype=np.float32)
    ones = np.ones((128, 128), np.float32)
    ltri = np.triu(np.ones((128, 128), np.float32), k=1)
    kp = np.arange(128)[:, None]
    col = np.arange(640)[None, :]
    kt = col // 128
    q = col % 128
    krel = kt * 128 + kp
    qrel = 512 + q
    cq = qrel // 64
    ck = krel // 64
    valid = (cq - ck >= 0) & (cq - ck <= 8)
    maskc = np.where(valid, 0.0, NEG).astype(np.float32)
    rel_idx = np.clip(qrel - krel, -128, 128) + 128
    ebase = np.broadcast_to((np.arange(32, dtype=np.float32) * CAP)[None, :], (128, 32)).copy()
    return ident, ones, ltri, maskc, rel_idx, ebase


def make_in_maps(inputs, cores=range(8), with_moe=True):
    f = lambda a: np.ascontiguousarray(np.asarray(a, dtype=np.float32))
    x = f(inputs["x"])
    ident, ones, ltri, maskc, rel_idx, ebase = make_consts()
    w_in = f(inputs["w_in"][0])
    w_out = f(inputs["w_out"][0])
    g1_bc = np.ascontiguousarray(np.broadcast_to(f(inputs["norm1_gain"][0])[None, :], (128, D)))
    g2_bc = np.ascontiguousarray(np.broadcast_to(f(inputs["norm2_gain"][0])[None, :], (128, D)))
    qg_col = f(inputs["q_norm_gain"][0]).reshape(128, 1)
    kg_col = f(inputs["k_norm_gain"][0]).reshape(128, 1)
    pscale = np.ascontiguousarray(f(inputs["pool_scale"][0]).reshape(8, 128).T)
    gw = f(inputs["pool_group_w"][0])
    gw_l = np.ascontiguousarray(gw.reshape(4, 2, 128, 256).transpose(2, 0, 1, 3).reshape(128, 4 * 2 * 256))
    rb = f(inputs["rel_bias"][0])
    biasT = np.ascontiguousarray(rb[:, rel_idx])
    wrg = f(inputs["w_router_group"][0])
    wre = f(inputs["w_router_expert"][0])
    wr = np.concatenate([wrg, wre.transpose(1, 0, 2).reshape(D, 32)], axis=1)
    wr_l = np.ascontiguousarray(wr.reshape(KC, 128, 36).transpose(1, 0, 2).reshape(128, KC * 36))
    br = np.concatenate([f(inputs["b_router_group"][0]), f(inputs["b_router_expert"][0]).reshape(32)])
    br_bc = np.ascontiguousarray(np.broadcast_to(br[None, :], (128, 36)))
    if with_moe:
        wg = f(inputs["w_expert_gate"][0]).reshape(NEXP * D, 1024)
        wu = f(inputs["w_expert_up"][0]).reshape(NEXP * D, 1024)
        wd = f(inputs["w_expert_down"][0]).reshape(NEXP * 1024, D)
    maps = []
    for c in cores:
        b, half = c // 2, c % 2
        xh = np.zeros((T_EXT, D), np.float32)
        if half == 1:
            xh[:] = x[b, T_OWN - HALO:2 * T_OWN]
        else:
            xh[HALO:] = x[b, 0:T_OWN]
        t_abs = np.arange(T_OWN) + half * T_OWN
        invc = np.stack([1.0 / np.minimum(t_abs + 1, w) for w in (2, 4, 8, 16)]).astype(np.float32)
        invc_bc = np.ascontiguousarray(np.broadcast_to(invc.reshape(1, 4 * T_OWN), (128, 4 * T_OWN)))
        halo_col = np.full((128, 1), 0.0 if half == 1 else NEG, np.float32)
        m = dict(xh=xh, w_in=w_in, w_out=w_out, g1_bc=g1_bc, g2_bc=g2_bc, qg_col=qg_col, kg_col=kg_col,
                 pscale=pscale, gw=gw_l, invc=invc_bc, biasT=biasT, maskc=maskc, halo_col=halo_col, wr=wr_l,
                 br_bc=br_bc, ebase_bc=ebase, ident=ident, ones=ones, ltri=ltri)
        if with_moe:
            m.update(wg=wg, wu=wu, wd=wd)
        maps.append(m)
    return maps


def kernel(**inputs):
    nc, _ = build_program("ABCDEFG", dbg=False)
    maps = make_in_maps(inputs)
    res = run_bass_kernel_spmd(nc, maps, core_ids=list(range(8)))
    outs = [np.asarray(r["out"]) for r in res.results]
    y = np.stack(outs, 0).reshape(4, 2 * T_OWN, D).astype(np.float32)
    return y
```

```python
import numpy as np
from contextlib import ExitStack
import concourse.bass as bass
import concourse.mybir as mybir
from concourse.bass_utils import run_bass_kernel_spmd

F32 = mybir.dt.float32
BF16 = mybir.dt.bfloat16
I32 = mybir.dt.int32
ALU = mybir.AluOpType
AF = mybir.ActivationFunctionType
AX = mybir.AxisListType
_DT_SIZE = {F32: 4, BF16: 2, I32: 4}

D = 4096
KC = 32
T_OWN = 2048
HALO = 512
T_EXT = T_OWN + HALO
NH = 24
CAP = 192
NEXP = 32
ZROW = NEXP * CAP
EPS = 1e-6
NEG = -30000.0


class Arena:
    def __init__(self, handle, nbytes, elem_bytes):
        self.h = handle
        self.nbytes = nbytes
        self.eb = elem_bytes
        self.top = 0
        self.marks = []

    def alloc(self, shape, dtype, align=64):
        sz = _DT_SIZE[dtype]
        n = 1
        for s in shape:
            n *= s
        nb = n * sz
        off = (self.top + align - 1) // align * align
        assert off + nb <= self.nbytes, f"arena overflow {off}+{nb}>{self.nbytes}"
        self.top = off + nb
        ap = self.h[:, off // self.eb:(off + nb) // self.eb]
        if dtype != self.h.dtype:
            ap = ap.bitcast(dtype)
        if len(shape) == 2:
            ap = ap.rearrange("p (a b) -> p a b", b=shape[1])
        elif len(shape) == 3:
            ap = ap.rearrange("p (a b c) -> p a b c", b=shape[1], c=shape[2])
        return ap

    def mark(self):
        self.marks.append(self.top)

    def release(self):
        self.top = self.marks.pop()


class Op:
    __slots__ = ("eng", "fn", "deps", "dma_key", "signal", "event")

    def __init__(self, eng, fn, dma_key):
        self.eng = eng
        self.fn = fn
        self.deps = set()
        self.dma_key = dma_key
        self.signal = dma_key is not None
        self.event = None


class Prog:
    def __init__(self, nc):
        self.nc = nc
        self.ops = []
        self.res = {}
        self.last_on = {}

    def add(self, eng, fn, reads=(), writes=(), accs=(), dma_key=None):
        idx = len(self.ops)
        op = Op(eng, fn, dma_key)
        deps = op.deps
        sk = ("dma", dma_key) if dma_key is not None else ("eng", eng)
        res = self.res
        for r in reads:
            st = res.get(r)
            if st is None:
                st = res[r] = {"pw": None, "aw": {}, "r": {}}
            if st["pw"] is not None:
                deps.add(st["pw"])
            deps.update(st["aw"].values())
        for w in writes:
            st = res.get(w)
            if st is None:
                st = res[w] = {"pw": None, "aw": {}, "r": {}}
            if st["pw"] is not None:
                deps.add(st["pw"])
            deps.update(st["aw"].values())
            deps.update(st["r"].values())
        for a in accs:
            st = res.get(a)
            if st is None:
                st = res[a] = {"pw": None, "aw": {}, "r": {}}
            if st["pw"] is not None:
                deps.add(st["pw"])
            deps.update(st["r"].values())
        for r in reads:
            res[r]["r"][sk] = idx
        for w in writes:
            st = res[w]
            st["pw"] = idx
            st["aw"] = {}
            st["r"] = {}
        for a in accs:
            res[a]["aw"][sk] = idx
        deps.discard(idx)
        self.ops.append(op)
        self.last_on[sk] = idx
        return idx

    def barrier(self):
        lasts = set(self.last_on.values())
        for eng in ("sp", "pe", "act", "dve", "pool"):
            op = Op(eng, None, None)
            op.deps = set(lasts)
            self.ops.append(op)
        self.res = {}

    def emit(self):
        nc = self.nc
        ops = self.ops
        for op in ops:
            nd = set()
            for d in op.deps:
                dop = ops[d]
                if dop.dma_key is None and op.dma_key is None and dop.eng == op.eng and op.eng == "pe":
                    continue
                nd.add(d)
            op.deps = nd
            for d in nd:
                ops[d].signal = True
        cnt = {}
        for op in ops:
            if op.dma_key is not None:
                k = ("dma", op.dma_key)
                cnt[k] = cnt.get(k, 0) + 16
                op.event = (k, cnt[k])
            elif op.signal:
                k = ("eng", op.eng)
                cnt[k] = cnt.get(k, 0) + 1
                op.event = (k, cnt[k])
        sem_keys = list(cnt.keys())
        self.n_sems = len(sem_keys)
        with ExitStack() as es:
            sems = {}
            for i, k in enumerate(sem_keys):
                sems[k] = es.enter_context(nc.semaphore(f"s{i}"))
            block = es.enter_context(nc.Block())
            engmap = {"sp": "sync", "pe": "tensor", "act": "scalar", "dve": "vector", "pool": "gpsimd"}

            def make(engname):
                def body(e):
                    seen = {}
                    for op in ops:
                        if op.eng != engname:
                            continue
                        for d in sorted(op.deps):
                            k, v = ops[d].event
                            if seen.get(k, 0) < v:
                                e.wait_ge(sems[k], v)
                                seen[k] = v
                        if op.fn is None:
                            continue
                        ins = op.fn(e)
                        if op.dma_key is not None:
                            ins.then_inc(sems[op.event[0]], 16)
                        elif op.signal:
                            ins.then_inc(sems[op.event[0]], 1)
                return body

            for engname, attr in engmap.items():
                getattr(block, attr)(make(engname))


def build_program(phases="ABCDEFG", dbg=False):
    nc = bass.Bass("TRN2", target_bir_lowering=False)

    def din(name, shape, dt=F32):
        return nc.dram_tensor(name, shape, dt, kind="ExternalInput").ap()

    skind = "ExternalOutput" if dbg else "Internal"

    def dscr(name, shape, dt):
        return nc.dram_tensor(name, shape, dt, kind=skind).ap()

    xh = din("xh", [T_EXT, D])
    w_in = din("w_in", [D, 10240])
    w_out = din("w_out", [D, D])
    g1_bc = din("g1_bc", [128, D])
    g2_bc = din("g2_bc", [128, D])
    qg_col = din("qg_col", [128, 1])
    kg_col = din("kg_col", [128, 1])
    pscale = din("pscale", [128, 8])
    gw_in = din("gw", [128, 4 * 2 * 256])
    invc_in = din("invc", [128, 4 * T_OWN])
    biasT = din("biasT", [NH, 128, 640])
    maskc_in = din("maskc", [128, 640])
    halo_in = din("halo_col", [128, 1])
    wr_in = din("wr", [128, KC * 36])
    br_in = din("br_bc", [128, 36])
    ebase_in = din("ebase_bc", [128, 32])
    ident_in = din("ident", [128, 128])
    ones_in = din("ones", [128, 128])
    ltri_in = din("ltri", [128, 128])
    use_moe = "F" in phases
    if use_moe:
        wg = din("wg", [NEXP * D, 1024])
        wu = din("wu", [NEXP * D, 1024])
        wd = din("wd", [NEXP * 1024, D])
    out = nc.dram_tensor("out", [T_OWN, D], F32, kind="ExternalOutput").ap()

    qT_s = dscr("qT_s", [NH * 128, T_OWN], BF16)
    kT_s = dscr("kT_s", [NH * 128, T_EXT], BF16)
    v_s = dscr("v_s", [T_EXT, 3072], BF16)
    uT_s = dscr("uT_s", [8 * 128, 2176], F32)
    x1_s = dscr("x1_s", [T_OWN, D], F32)
    Hs = dscr("Hs", [NEXP * CAP, D], BF16)
    Ys = dscr("Ys", [NEXP * CAP + 128, D], F32)
    rt_dbg = dscr("rt_dbg", [128, 16 * 4], F32)

    SB_BYTES = 212000
    sb_h = nc.alloc_sbuf_tensor("arena", [128, SB_BYTES // 2], BF16)
    ps_h = nc.alloc_psum_tensor("psarena", [128, 4096], F32)
    A = Arena(sb_h, SB_BYTES, 2)
    P = Prog(nc)

    def bank(b, n=512, dt=F32):
        ap = ps_h[:, b * 512:(b + 1) * 512]
        if dt == BF16:
            return ap.bitcast(BF16)[:, 0:n]
        return ap[:, 0:n]

    ident_bf = A.alloc([128], BF16)
    ident_f = A.alloc([128], F32)
    ones_bf = A.alloc([128], BF16)
    ltri_bf = A.alloc([128], BF16)
    qg = A.alloc([2], F32)
    kg = A.alloc([2], F32)
    halo = A.alloc([2], F32)
    slot_i = A.alloc([16, 2], I32)
    gidx_i = A.alloc([16, 2], I32)
    gate_all = A.alloc([16, 2], F32)

    P.add("pool", lambda e: e.dma_start(out=ident_bf, in_=ident_in), writes=["ident_bf"], dma_key="c0")
    P.add("pool", lambda e: e.dma_start(out=ones_bf, in_=ones_in), writes=["ones_bf"], dma_key="c1")
    P.add("pool", lambda e: e.dma_start(out=ltri_bf, in_=ltri_in), writes=["ltri_bf"], dma_key="c2")
    P.add("sp", lambda e: e.dma_start(out=ident_f, in_=ident_in), writes=["ident_f"], dma_key="c3")
    P.add("sp", lambda e: e.dma_start(out=qg[:, 0:1], in_=qg_col), writes=["qg0"], dma_key="c4")
    P.add("sp", lambda e: e.dma_start(out=kg[:, 0:1], in_=kg_col), writes=["kg0"], dma_key="c5")
    P.add("sp", lambda e: e.dma_start(out=halo[:, 0:1], in_=halo_in), writes=["halo"], dma_key="c6")
    P.add("dve", lambda e: e.tensor_scalar(out=qg[:, 1:2], in0=qg[:, 0:1], scalar1=float(128 ** -0.5), scalar2=None,
                                           op0=ALU.mult), reads=["qg0"], writes=["qg"])
    P.add("dve", lambda e: e.tensor_copy(out=kg[:, 1:2], in_=kg[:, 0:1]), reads=["kg0"], writes=["kg"])
    if "A" in phases:
        A.mark()
        hT = A.alloc([KC, 1280], BF16)
        gbc = A.alloc([D], F32)
        G = 512
        NS = 2
        wb = [A.alloc([KC, G], BF16), None]
        w_in_v = w_in.rearrange("(kc p) n -> p kc n", p=128)

        def load_w(gi):
            s = gi % NS
            P.add("pool", lambda e, gi=gi, s=s: e.dma_start(out=wb[s], in_=w_in_v[:, :, gi * G:(gi + 1) * G]),
                  writes=[("wb", s)], dma_key=("wb", s))

        P.add("sp", lambda e: e.dma_start(out=gbc, in_=g1_bc), writes=["gbc"], dma_key="gbc")
        for hh in range(2):
            load_w(0)
            A.mark()
            xs = [A.alloc([D], F32) for _ in range(2)]
            xn = [A.alloc([D], BF16) for _ in range(2)]
            junkA = A.alloc([D], BF16)
            st = [A.alloc([4], F32) for _ in range(2)]
            for i in range(10):
                ti = hh * 10 + i
                s = i % 2
                P.add("sp", lambda e, ti=ti, s=s: e.dma_start(out=xs[s], in_=xh[ti * 128:(ti + 1) * 128, :]),
                      writes=[("xs", s)], dma_key=("xs", s))
                P.add("act", lambda e, s=s, junkA=junkA: e.activation(out=junkA, in_=xs[s], func=AF.Square,
                                                                       accum_out=st[s][:, 0:1]),
                      reads=[("xs", s)], writes=["junkA", ("st0", s)])
                P.add("act", lambda e, s=s: e.activation(out=st[s][:, 1:2], in_=st[s][:, 0:1], func=AF.Sqrt,
                                                         scale=1.0 / D, bias=EPS),
                      reads=[("st0", s)], writes=[("st1", s)])
                P.add("dve", lambda e, s=s: e.reciprocal(out=st[s][:, 2:3], in_=st[s][:, 1:2]),
                      reads=[("st1", s)], writes=[("st2", s)])
                P.add("dve", lambda e, s=s: e.scalar_tensor_tensor(out=xn[s], in0=xs[s], scalar=st[s][:, 2:3], in1=gbc,
                                                                   op0=ALU.mult, op1=ALU.mult),
                      reads=[("xs", s), ("st2", s), "gbc"], writes=[("xn", s)])
                for c in range(KC):
                    pb = 6 + (c // 4) % 2
                    P.add("pe", lambda e, c=c, s=s, pb=pb: e.transpose(
                        out=bank(pb, 512, BF16)[:, (c % 4) * 128:(c % 4 + 1) * 128],
                        in_=xn[s][:, c * 128:(c + 1) * 128], identity=ident_bf),
                        reads=[("xn", s), "ident_bf"], writes=[("ps", pb)])
                    if c % 4 == 3:
                        c0 = c - 3
                        src = bank(pb, 512, BF16).rearrange("p (a b) -> p a b", b=128)
                        if (c // 4) % 2:
                            P.add("dve", lambda e, c0=c0, i=i, src=src: e.tensor_copy(
                                out=hT[:, c0:c0 + 4, i * 128:(i + 1) * 128], in_=src),
                                reads=[("ps", pb)], accs=[("hT", i)])
                        else:
                            P.add("act", lambda e, c0=c0, i=i, src=src: e.copy(
                                out=hT[:, c0:c0 + 4, i * 128:(i + 1) * 128], in_=src),
                                reads=[("ps", pb)], accs=[("hT", i)])
            P.barrier()
            A.release()
            A.mark()
            wb[1] = A.alloc([KC, G], BF16)
            sq = [A.alloc([512], BF16) for _ in range(3)]
            rt = [A.alloc([512], F32) for _ in range(3)]
            qo = [A.alloc([512], BF16) for _ in range(3)]
            uo = [A.alloc([512], F32) for _ in range(2)]
            vo = [A.alloc([512], BF16) for _ in range(2)]
            ngroups = 20
            blk = [0]
            pending = []

            def flush():
                while pending:
                    pending.pop(0)()

            for gi in range(ngroups):
                if gi + 1 < ngroups:
                    load_w(gi + 1)
                s = gi % NS
                wkey = ("wb", s)
                if gi >= 14:
                    flush()
                    vc0 = (gi - 14) * 512
                    for i in range(10):
                        b = blk[0] % 3
                        blk[0] += 1
                        for kc in range(KC):
                            P.add("pe", lambda e, kc=kc, i=i, b=b, s=s: e.matmul(
                                bank(b), lhsT=hT[:, kc, i * 128:(i + 1) * 128], rhs=wb[s][:, kc, :],
                                start=(kc == 0), stop=(kc == KC - 1)),
                                reads=[wkey, ("hT", i)], writes=[("ps", b)])
                        vs_ = i % 2
                        ti = hh * 10 + i
                        if i % 2:
                            P.add("dve", lambda e, b=b, vs_=vs_: e.tensor_copy(out=vo[vs_], in_=bank(b)),
                                  reads=[("ps", b)], writes=[("vo", vs_)])
                        else:
                            P.add("act", lambda e, b=b, vs_=vs_: e.copy(out=vo[vs_], in_=bank(b)),
                                  reads=[("ps", b)], writes=[("vo", vs_)])
                        P.add("sp", lambda e, vs_=vs_, ti=ti, vc0=vc0: e.dma_start(
                            out=v_s[ti * 128:(ti + 1) * 128, vc0:vc0 + 512], in_=vo[vs_]),
                            reads=[("vo", vs_)], accs=["v_s"], dma_key=("vo", vs_))
                    continue
                if gi < 2:
                    kind = "u"
                    lo = 384 if hh == 0 else 0
                elif gi < 8:
                    kind = "q"
                    lo = 512 if hh == 0 else 0
                else:
                    kind = "k"
                    lo = 0
                nblocks = []
                n0 = lo
                while n0 < 1280:
                    nsz = min(512, 1280 - n0)
                    nblocks.append((n0, nsz))
                    n0 += nsz
                for ct in range(4):
                    for (n0, nsz) in nblocks:
                        b = blk[0] % 3
                        blk[0] += 1
                        tiles = list(range(n0 // 128, (n0 + nsz + 127) // 128))
                        for kc in range(KC):
                            P.add("pe", lambda e, kc=kc, ct=ct, n0=n0, nsz=nsz, b=b, s=s: e.matmul(
                                bank(b, nsz), lhsT=wb[s][:, kc, ct * 128:(ct + 1) * 128], rhs=hT[:, kc, n0:n0 + nsz],
                                start=(kc == 0), stop=(kc == KC - 1)),
                                reads=[wkey] + [("hT", j) for j in tiles], writes=[("ps", b)])
                        ext0 = hh * 1280 + n0
                        if kind == "u":
                            c = gi * 4 + ct
                            us = blk[0] % 2
                            P.add("act", lambda e, b=b, nsz=nsz, us=us: e.copy(out=uo[us][:, 0:nsz], in_=bank(b, nsz)),
                                  reads=[("ps", b)], writes=[("uo", us)])
                            P.add("sp", lambda e, c=c, us=us, nsz=nsz, ext0=ext0: e.dma_start(
                                out=uT_s[c * 128:(c + 1) * 128, ext0 - 384:ext0 - 384 + nsz], in_=uo[us][:, 0:nsz]),
                                reads=[("uo", us)], accs=["uT_s"], dma_key=("uo", us))
                            continue
                        if kind == "q":
                            h = (gi - 2) * 4 + ct
                            gcol = qg[:, 1:2]
                            gk = "qg"
                            dst = qT_s[h * 128:(h + 1) * 128, ext0 - 512:ext0 - 512 + nsz]
                            dkey = "qT_s"
                        else:
                            h = (gi - 8) * 4 + ct
                            gcol = kg[:, 1:2]
                            gk = "kg"
                            dst = kT_s[h * 128:(h + 1) * 128, ext0:ext0 + nsz]
                            dkey = "kT_s"
                        r = (blk[0] - 1) % 3
                        b2 = 3 + (blk[0] - 1) % 2
                        P.add("act", lambda e, b=b, nsz=nsz, r=r: e.activation(out=sq[r][:, 0:nsz], in_=bank(b, nsz),
                                                                              func=AF.Square),
                              reads=[("ps", b)], writes=[("sq", r)])

                        def stage2(b=b, nsz=nsz, r=r, b2=b2, gcol=gcol, gk=gk, dst=dst, dkey=dkey):
                            P.add("pe", lambda e: e.matmul(bank(b2, nsz), lhsT=ones_bf, rhs=sq[r][:, 0:nsz],
                                                           start=True, stop=True),
                                  reads=["ones_bf", ("sq", r)], writes=[("ps", b2)])
                            P.add("act", lambda e: e.activation(out=rt[r][:, 0:nsz], in_=bank(b2, nsz), func=AF.Sqrt,
                                                                scale=1.0 / 128, bias=EPS),
                                  reads=[("ps", b2)], writes=[("rt", r)])
                            P.add("dve", lambda e: e.reciprocal(out=rt[r][:, 0:nsz], in_=rt[r][:, 0:nsz]),
                                  reads=[("rt", r)], writes=[("rt", r)])
                            P.add("dve", lambda e: e.scalar_tensor_tensor(
                                out=qo[r][:, 0:nsz], in0=bank(b, nsz), scalar=gcol, in1=rt[r][:, 0:nsz],
                                op0=ALU.mult, op1=ALU.mult),
                                reads=[("ps", b), ("rt", r), gk], writes=[("qo", r)])
                            P.add("sp", lambda e: e.dma_start(out=dst, in_=qo[r][:, 0:nsz]),
                                  reads=[("qo", r)], accs=[dkey], dma_key=("qo", r))

                        flush()
                        pending.append(stage2)
            flush()
            P.barrier()
            A.release()
        A.release()

    if "C" in phases:
        A.mark()
        yT = A.alloc([KC, T_OWN], BF16)
        A.mark()
        maskc = A.alloc([640], F32)
        qh = [A.alloc([T_OWN], BF16) for _ in range(2)]
        kh = [A.alloc([T_EXT], BF16) for _ in range(2)]
        vh = [A.alloc([20, 130], BF16) for _ in range(2)]
        bh = [A.alloc([640], F32) for _ in range(2)]
        bhi = [A.alloc([640], BF16) for _ in range(2)]
        blo = [A.alloc([640], BF16) for _ in range(2)]
        Pb = [A.alloc([640], BF16) for _ in range(2)]
        rzc = [A.alloc([2], F32) for _ in range(2)]
        ytm = [A.alloc([128], BF16) for _ in range(2)]
        P.add("sp", lambda e: e.dma_start(out=maskc, in_=maskc_in), writes=["maskc"], dma_key="maskc")
        for s_ in range(2):
            P.add("dve", lambda e, s_=s_: e.memset(vh[s_][:, :, 128:130], 1.0), accs=[("vh", s_)])
        v_v = v_s.rearrange("(t p) c -> p t c", p=128)

        def load_head(h):
            s = h % 2
            P.add("sp", lambda e, h=h, s=s: e.dma_start(out=qh[s], in_=qT_s[h * 128:(h + 1) * 128, :]),
                  reads=["qT_s"], writes=[("qh", s)], dma_key=("qh", s))
            P.add("sp", lambda e, h=h, s=s: e.dma_start(out=kh[s], in_=kT_s[h * 128:(h + 1) * 128, :]),
                  reads=["kT_s"], writes=[("kh", s)], dma_key=("kh", s))
            P.add("sp", lambda e, h=h, s=s: e.dma_start(out=vh[s][:, :, 0:128], in_=v_v[:, :, h * 128:(h + 1) * 128]),
                  reads=["v_s"], accs=[("vh", s)], dma_key=("vh", s))
            P.add("sp", lambda e, h=h, s=s: e.dma_start(out=bh[s], in_=biasT[h]),
                  writes=[("bh0", s)], dma_key=("bh", s))

        def fix_head(h):
            s = h % 2
            P.add("dve", lambda e, s=s: e.tensor_tensor(out=bh[s], in0=bh[s], in1=maskc, op=ALU.add),
                  reads=[("bh0", s), "maskc"], writes=[("bh", s)])
            P.add("dve", lambda e, s=s: e.tensor_copy(out=bhi[s], in_=bh[s]), reads=[("bh", s)], writes=[("bhi", s)])
            P.add("dve", lambda e, s=s: e.tensor_tensor(out=blo[s], in0=bh[s], in1=bhi[s], op=ALU.subtract),
                  reads=[("bh", s), ("bhi", s)], writes=[("blo", s)])

        iters = [(h, t) for h in range(NH) for t in range(16)]
        NI = len(iters)

        def S_of(u):
            return ps_h[:, 2 * u * 512: 2 * u * 512 + 640]

        def rec_qk(it):
            h, t = iters[it]
            s = h % 2
            u = it % 2
            S = S_of(u)
            for kt in range(5):
                Sk = S[:, kt * 128:(kt + 1) * 128]
                P.add("pe", lambda e, kt=kt, t=t, s=s, Sk=Sk: e.matmul(
                    Sk, lhsT=kh[s][:, (t + kt) * 128:(t + kt + 1) * 128],
                    rhs=qh[s][:, t * 128:(t + 1) * 128], start=True, stop=False),
                    reads=[("kh", s), ("qh", s)], writes=[("S", u)])
                P.add("pe", lambda e, kt=kt, s=s, Sk=Sk: e.matmul(
                    Sk, lhsT=ident_bf, rhs=bhi[s][:, kt * 128:(kt + 1) * 128], start=False, stop=False),
                    reads=["ident_bf", ("bhi", s)], writes=[("S", u)])
                P.add("pe", lambda e, kt=kt, s=s, Sk=Sk: e.matmul(
                    Sk, lhsT=ident_bf, rhs=blo[s][:, kt * 128:(kt + 1) * 128], start=False, stop=True),
                    reads=["ident_bf", ("blo", s)], writes=[("S", u)])

        def rec_softmax(it):
            h, t = iters[it]
            u = it % 2
            S = S_of(u)
            nh_ = max(0, 4 - t)
            if nh_ > 0:
                P.add("act", lambda e, u=u, nh_=nh_, S=S: e.activation(out=Pb[u][:, 0:nh_ * 128], in_=S[:, 0:nh_ * 128],
                                                                      func=AF.Exp, bias=halo[:, 0:1]),
                      reads=[("S", u), "halo"], accs=[("Pb", u)])
            P.add("act", lambda e, u=u, nh_=nh_, S=S: e.activation(out=Pb[u][:, nh_ * 128:640], in_=S[:, nh_ * 128:640],
                                                                  func=AF.Exp),
                  reads=[("S", u)], accs=[("Pb", u)])

        def rec_pv(it):
            h, t = iters[it]
            s = h % 2
            u = it % 2
            O = bank(4 + u)[:, 0:129]
            for kt in range(5):
                P.add("pe", lambda e, kt=kt, t=t, s=s, u=u, O=O: e.matmul(
                    O, lhsT=Pb[u][:, kt * 128:(kt + 1) * 128], rhs=vh[s][:, t + kt, 0:129],
                    start=(kt == 0), stop=(kt == 4)),
                    reads=[("vh", s), ("Pb", u)], writes=[("OZ", u)])
            P.add("dve", lambda e, u=u, O=O: e.reciprocal(out=rzc[u][:, 0:1], in_=O[:, 128:129]),
                  reads=[("OZ", u)], writes=[("rzc", u)])
            P.add("dve", lambda e, u=u, O=O: e.tensor_scalar(out=ytm[u], in0=O[:, 0:128], scalar1=rzc[u][:, 0:1],
                                                             scalar2=None, op0=ALU.mult),
                  reads=[("OZ", u), ("rzc", u)], writes=[("ytm", u)])

        def rec_tr(it):
            h, t = iters[it]
            u = it % 2
            tb = 6 + u
            TR = bank(tb, 512, BF16)[:, 0:128]
            P.add("pe", lambda e, u=u, TR=TR: e.transpose(out=TR, in_=ytm[u], identity=ident_bf),
                  reads=[("ytm", u), "ident_bf"], writes=[("ps", tb)])
            P.add("dve", lambda e, TR=TR, h=h, t=t: e.tensor_copy(out=yT[:, 8 + h, t * 128:(t + 1) * 128], in_=TR),
                  reads=[("ps", tb)], accs=["yT"])

        load_head(0)
        fix_head(0)
        rec_qk(0)
        rec_softmax(0)
        rec_qk(1)
        for it in range(NI):
            h, t = iters[it]
            if t == 0 and h + 1 < NH:
                load_head(h + 1)
            if t == 10 and h + 1 < NH:
                fix_head(h + 1)
            if it + 1 < NI:
                rec_softmax(it + 1)
            if it + 2 < NI:
                rec_qk(it + 2)
            rec_pv(it)
            if it >= 1:
                rec_tr(it - 1)
        rec_tr(NI - 1)
        P.barrier()
        A.release()

        A.mark()
        gwb = A.alloc([4, 2, 256], BF16)
        psc = A.alloc([8], F32)
        ut = [A.alloc([2176], F32) for _ in range(2)]
        sa = A.alloc([2176], F32)
        sbuf_b = A.alloc([2176], F32)
        invc = [A.alloc([T_OWN], F32) for _ in range(1)]
        pooled = [A.alloc([2, T_OWN], BF16) for _ in range(1)]
        P.add("pool", lambda e: e.dma_start(out=gwb, in_=gw_in.rearrange("p (g k n) -> p g k n", g=4, k=2)),
              writes=["gwb"], dma_key="gwb")
        P.add("sp", lambda e: e.dma_start(out=psc, in_=pscale), writes=["psc"], dma_key="psc")
        invc_v = invc_in.rearrange("p (g t) -> p g t", g=4)
        ev = 0
        for g in range(4):
            gs = 0
            P.add("sp", lambda e, g=g, gs=gs: e.dma_start(out=invc[gs], in_=invc_v[:, g, :]),
                  writes=[("invc", gs)], dma_key=("invc", gs))
            for kc2 in range(2):
                c = g * 2 + kc2
                us = c % 2
                P.add("sp", lambda e, c=c, us=us: e.dma_start(out=ut[us], in_=uT_s[c * 128:(c + 1) * 128, :]),
                      reads=["uT_s"], writes=[("ut", us)], dma_key=("ut", us))
                cur = ut[us]
                curk = ("ut", us)
                bufs = [(sa, "sa"), (sbuf_b, "sbb")]
                for j in range(g + 1):
                    sh = 1 << j
                    dstb, dk = bufs[j % 2]
                    P.add("dve", lambda e, cur=cur, dstb=dstb, sh=sh: e.tensor_tensor(
                        out=dstb[:, 16:2176], in0=cur[:, 16:2176], in1=cur[:, 16 - sh:2176 - sh], op=ALU.add),
                        reads=[curk], writes=[dk])
                    cur, curk = dstb, dk
                other, ok = bufs[(g + 1) % 2]
                P.add("dve", lambda e, cur=cur, other=other, gs=gs: e.tensor_tensor(
                    out=other[:, 128:2176], in0=cur[:, 128:2176], in1=invc[gs], op=ALU.mult),
                    reads=[curk, ("invc", gs)], writes=[ok])
                P.add("dve", lambda e, other=other, us=us, gs=gs, kc2=kc2: e.tensor_tensor(
                    out=pooled[gs][:, kc2, :], in0=other[:, 128:2176], in1=ut[us][:, 128:2176], op=ALU.subtract),
                    reads=[ok, ("ut", us)], accs=[("pooled", gs)])
            for oc in range(2):
                for nb in range(4):
                    b = ev % 2
                    ev += 1
                    for kc2 in range(2):
                        P.add("pe", lambda e, g=g, kc2=kc2, oc=oc, nb=nb, b=b, gs=gs: e.matmul(
                            bank(b), lhsT=gwb[:, g, kc2, oc * 128:(oc + 1) * 128],
                            rhs=pooled[gs][:, kc2, nb * 512:(nb + 1) * 512], start=(kc2 == 0), stop=(kc2 == 1)),
                            reads=["gwb", ("pooled", gs)], writes=[("ps", b)])
                    cc = g * 2 + oc
                    P.add("act", lambda e, cc=cc, nb=nb, b=b: e.activation(
                        out=yT[:, cc, nb * 512:(nb + 1) * 512], in_=bank(b), func=AF.Copy, scale=psc[:, cc:cc + 1]),
                        reads=[("ps", b), "psc"], accs=["yT"])
        P.barrier()
        A.release()

        A.mark()
        G2 = 512
        wob = [A.alloc([KC, G2], BF16) for _ in range(2)]
        xt = [A.alloc([G2], F32) for _ in range(3)]
        w_out_v = w_out.rearrange("(kc p) n -> p kc n", p=128)

        def load_wo(j):
            s = j % 2
            P.add("pool", lambda e, j=j, s=s: e.dma_start(out=wob[s], in_=w_out_v[:, :, j * G2:(j + 1) * G2]),
                  writes=[("wob", s)], dma_key=("wob", s))

        load_wo(0)
        it = 0
        for j in range(D // G2):
            if j + 1 < D // G2:
                load_wo(j + 1)
            s = j % 2
            for i in range(16):
                b = it % 2
                r = it % 3
                it += 1
                P.add("sp", lambda e, i=i, j=j, r=r: e.dma_start(
                    out=xt[r], in_=xh[(4 + i) * 128:(5 + i) * 128, j * G2:(j + 1) * G2]),
                    writes=[("xt", r)], dma_key=("xt", r))
                for kc in range(KC):
                    P.add("pe", lambda e, kc=kc, i=i, b=b, s=s: e.matmul(
                        bank(b, G2), lhsT=yT[:, kc, i * 128:(i + 1) * 128], rhs=wob[s][:, kc, :],
                        start=(kc == 0), stop=(kc == KC - 1)),
                        reads=["yT", ("wob", s)], writes=[("ps", b)])
                P.add("dve", lambda e, b=b, r=r: e.tensor_tensor(out=xt[r], in0=bank(b, G2), in1=xt[r], op=ALU.add),
                      reads=[("ps", b), ("xt", r)], writes=[("xt", r)])
                P.add("pool", lambda e, i=i, j=j, r=r: e.dma_start(
                    out=x1_s[i * 128:(i + 1) * 128, j * G2:(j + 1) * G2], in_=xt[r]),
                    reads=[("xt", r)], accs=["x1_s"], dma_key=("xo", r))
        P.barrier()
        A.release()
        A.release()

    if "E" in phases:
        A.mark()
        gbc2 = A.alloc([D], F32)
        wr = A.alloc([KC, 36], F32)
        brb = A.alloc([36], F32)
        ebase = A.alloc([32], F32)
        carry = A.alloc([32], F32)
        x1t = [A.alloc([D], F32) for _ in range(2)]
        h2 = [A.alloc([D], F32) for _ in range(2)]
        h2b = [A.alloc([D], BF16) for _ in range(3)]
        h2T = [A.alloc([KC, 128], F32) for _ in range(2)]
        st2 = [A.alloc([4], F32) for _ in range(2)]
        rs = [A.alloc([320], F32) for _ in range(2)]
        Mb = [A.alloc([32], BF16) for _ in range(2)]
        junkE = A.alloc([D], BF16)
        P.add("sp", lambda e: e.dma_start(out=gbc2, in_=g2_bc), writes=["gbc2"], dma_key="gbc2")
        P.add("sp", lambda e: e.dma_start(out=wr, in_=wr_in.rearrange("p (k j) -> p k j", j=36)), writes=["wr"], dma_key="wr")
        P.add("sp", lambda e: e.dma_start(out=brb, in_=br_in), writes=["brb"], dma_key="brb")
        P.add("sp", lambda e: e.dma_start(out=ebase, in_=ebase_in), writes=["ebase"], dma_key="ebase")
        P.add("dve", lambda e: e.memset(carry, 0.0), writes=["carry"])
        if use_moe:
            zt = A.alloc([1024], F32)
            P.add("dve", lambda e: e.memset(zt, 0.0), writes=["zt"])
            for q4 in range(4):
                P.add("sp", lambda e, q4=q4: e.dma_start(out=Ys[ZROW:ZROW + 128, q4 * 1024:(q4 + 1) * 1024], in_=zt),
                      reads=["zt"], accs=["Ys"], dma_key="zf2")

        def e_stage1(i):
            s = i % 2
            P.add("sp", lambda e, i=i, s=s: e.dma_start(out=x1t[s], in_=x1_s[i * 128:(i + 1) * 128, :]),
                  reads=["x1_s"], writes=[("x1t", s)], dma_key=("x1t", s))
            P.add("act", lambda e, s=s: e.activation(out=junkE, in_=x1t[s], func=AF.Square, accum_out=st2[s][:, 0:1]),
                  reads=[("x1t", s)], writes=["junkE", ("s0", s)])
            P.add("act", lambda e, s=s: e.activation(out=st2[s][:, 1:2], in_=st2[s][:, 0:1], func=AF.Sqrt,
                                                     scale=1.0 / D, bias=EPS), reads=[("s0", s)], writes=[("s1", s)])
            P.add("dve", lambda e, s=s: e.reciprocal(out=st2[s][:, 2:3], in_=st2[s][:, 1:2]),
                  reads=[("s1", s)], writes=[("s2", s)])
            P.add("dve", lambda e, s=s: e.scalar_tensor_tensor(out=h2[s], in0=x1t[s], scalar=st2[s][:, 2:3], in1=gbc2,
                                                               op0=ALU.mult, op1=ALU.mult),
                  reads=[("x1t", s), ("s2", s), "gbc2"], writes=[("h2", s)])
            P.add("act", lambda e, s=s, i=i: e.copy(out=h2b[i % 3], in_=h2[s]), reads=[("h2", s)], writes=[("h2b", i % 3)])
            for c in range(KC):
                pb = 6 + (c // 4) % 2
                P.add("pe", lambda e, c=c, pb=pb, s=s: e.transpose(out=bank(pb)[:, (c % 4) * 128:(c % 4 + 1) * 128],
                                                                    in_=h2[s][:, c * 128:(c + 1) * 128], identity=ident_f),
                      reads=[("h2", s), "ident_f"], writes=[("ps", pb)])
                if c % 4 == 3:
                    c0 = c - 3
                    src = bank(pb).rearrange("p (a b) -> p a b", b=128)
                    if (c // 4) % 2:
                        P.add("dve", lambda e, c0=c0, src=src, s=s: e.tensor_copy(out=h2T[s][:, c0:c0 + 4, :], in_=src),
                              reads=[("ps", pb)], accs=[("h2T", s)])
                    else:
                        P.add("act", lambda e, c0=c0, src=src, s=s: e.copy(out=h2T[s][:, c0:c0 + 4, :], in_=src),
                              reads=[("ps", pb)], accs=[("h2T", s)])
            lg = bank(s, 36)
            for kc in range(KC):
                P.add("pe", lambda e, kc=kc, lg=lg, s=s: e.matmul(lg, lhsT=h2T[s][:, kc, :], rhs=wr[:, kc, :],
                                                                  start=(kc == 0), stop=(kc == KC - 1)),
                      reads=[("h2T", s), "wr"], writes=[("ps", s)])

        def e_stage2(i):
            s = i % 2
            lg = bank(s, 36)
            R = rs[s]
            L = R[:, 0:36]
            mg = R[:, 36:37]
            ohg = R[:, 40:44]
            nmg = R[:, 44:45]
            eg = R[:, 48:52]
            se = R[:, 52:53]
            psel = R[:, 53:54]
            pen = R[:, 56:60]
            Lem = R[:, 64:96]
            m1 = R[:, 96:97]
            m2 = R[:, 97:98]
            dd = R[:, 98:99]
            e2 = R[:, 99:100]
            t1 = R[:, 100:101]
            g1 = R[:, 101:102]
            g2 = R[:, 102:103]
            oh1 = R[:, 104:136]
            Lem2 = R[:, 136:168]
            oh2 = R[:, 168:200]
            Rk = R[:, 200:232]
            ov = R[:, 232:264]
            sv = R[:, 264:296]
            sl = R[:, 296:298]
            slg = R[:, 298:300]
            rk = ("rs", s)

            def dv(fn, reads=(), writes=()):
                P.add("dve", fn, reads=[rk] + list(reads), writes=[rk] + list(writes))

            dv(lambda e: e.tensor_tensor(out=L, in0=lg, in1=brb, op=ALU.add), reads=[("ps", s), "brb"])
            dv(lambda e: e.reduce_max(out=mg, in_=L[:, 0:4], axis=AX.X))
            dv(lambda e: e.tensor_scalar(out=ohg, in0=L[:, 0:4], scalar1=mg, scalar2=None, op0=ALU.is_equal))
            dv(lambda e: e.tensor_scalar(out=nmg, in0=mg, scalar1=-1.0, scalar2=None, op0=ALU.mult))
            P.add("act", lambda e: e.activation(out=eg, in_=L[:, 0:4], func=AF.Exp, bias=nmg, accum_out=se),
                  reads=[rk], writes=[rk])
            dv(lambda e: e.reciprocal(out=psel, in_=se))
            dv(lambda e: e.tensor_scalar(out=pen, in0=ohg, scalar1=-1.0, scalar2=1e30, op0=ALU.add, op1=ALU.mult))
            for g in range(4):
                dv(lambda e, g=g: e.tensor_scalar(
                    out=Lem[:, g * 8:(g + 1) * 8], in0=L[:, 4 + g * 8:12 + g * 8], scalar1=pen[:, g:g + 1], scalar2=None,
                    op0=ALU.add))
            dv(lambda e: e.reduce_max(out=m1, in_=Lem, axis=AX.X))
            dv(lambda e: e.tensor_scalar(out=oh1, in0=Lem, scalar1=m1, scalar2=None, op0=ALU.is_equal))
            dv(lambda e: e.scalar_tensor_tensor(out=Lem2, in0=oh1, scalar=-1e30, in1=Lem, op0=ALU.mult, op1=ALU.add))
            dv(lambda e: e.reduce_max(out=m2, in_=Lem2, axis=AX.X))
            dv(lambda e: e.tensor_scalar(out=oh2, in0=Lem2, scalar1=m2, scalar2=None, op0=ALU.is_equal))
            dv(lambda e: e.tensor_tensor(out=dd, in0=m2, in1=m1, op=ALU.subtract))
            P.add("act", lambda e: e.activation(out=e2, in_=dd, func=AF.Exp), reads=[rk], writes=[rk])
            dv(lambda e: e.tensor_scalar(out=t1, in0=e2, scalar1=1.0, scalar2=None, op0=ALU.add))
            dv(lambda e: e.reciprocal(out=g1, in_=t1))
            dv(lambda e: e.tensor_tensor(out=gate_all[:, i, 0:1], in0=g1, in1=psel, op=ALU.mult), writes=[("gate_all", i)])
            dv(lambda e: e.tensor_tensor(out=g2, in0=e2, in1=g1, op=ALU.mult))
            dv(lambda e: e.tensor_tensor(out=gate_all[:, i, 1:2], in0=g2, in1=psel, op=ALU.mult), writes=[("gate_all", i)])
            P.add("dve", lambda e: e.tensor_tensor(out=Mb[s], in0=oh1, in1=oh2, op=ALU.add),
                  reads=[rk], writes=[("Mb", s)])
            rkb = bank(2, 64)
            P.add("pe", lambda e: e.matmul(rkb[:, 0:32], lhsT=ltri_bf, rhs=Mb[s], start=True, stop=True),
                  reads=["ltri_bf", ("Mb", s)], writes=[("ps", 2)])
            P.add("pe", lambda e: e.matmul(rkb[:, 32:64], lhsT=ones_bf, rhs=Mb[s], start=True, stop=True),
                  reads=["ones_bf", ("Mb", s)], writes=[("ps", 2)])
            dv(lambda e: e.tensor_tensor(out=Rk, in0=rkb[:, 0:32], in1=carry, op=ALU.add), reads=[("ps", 2), "carry"])
            P.add("dve", lambda e: e.tensor_tensor(out=carry, in0=rkb[:, 32:64], in1=carry, op=ALU.add),
                  reads=[("ps", 2), "carry", rk], writes=["carry"])
            dv(lambda e: e.tensor_scalar(out=ov, in0=Rk, scalar1=float(CAP), scalar2=1e6, op0=ALU.is_ge, op1=ALU.mult))
            dv(lambda e: e.tensor_tensor(out=sv, in0=Rk, in1=ebase, op=ALU.add), reads=["ebase"])
            dv(lambda e: e.tensor_tensor(out=sv, in0=sv, in1=ov, op=ALU.add))
            for k, oh in ((0, oh1), (1, oh2)):
                dv(lambda e, oh=oh: e.tensor_tensor(out=ov, in0=oh, in1=sv, op=ALU.mult))
                dv(lambda e, k=k: e.reduce_sum(out=sl[:, k:k + 1], in_=ov, axis=AX.X))
            dv(lambda e: e.tensor_copy(out=slot_i[:, i, :], in_=sl), writes=[("slot_i", i)])
            dv(lambda e: e.tensor_scalar(out=slg, in0=sl, scalar1=float(ZROW), scalar2=None, op0=ALU.min))
            dv(lambda e: e.tensor_copy(out=gidx_i[:, i, :], in_=slg), writes=[("gidx_i", i)])
            if dbg:
                P.add("sp", lambda e: e.dma_start(out=rt_dbg[:, i * 4:i * 4 + 2], in_=sl),
                      reads=[rk], accs=["rt_dbg"], dma_key=("dbg", s))
                P.add("sp", lambda e: e.dma_start(out=rt_dbg[:, i * 4 + 2:i * 4 + 4], in_=gate_all[:, i, :]),
                      reads=[("gate_all", i)], accs=["rt_dbg"], dma_key=("dbg2", s))
            if use_moe:
                for k in range(2):
                    P.add("pool", lambda e, k=k: e.indirect_dma_start(
                        out=Hs[:, :], out_offset=bass.IndirectOffsetOnAxis(ap=slot_i[:, i, k:k + 1], axis=0),
                        in_=h2b[i % 3], in_offset=None, bounds_check=NEXP * CAP - 1, oob_is_err=False),
                        reads=[("h2b", i % 3), ("slot_i", i)], accs=["Hs"], dma_key=("sc", i % 3, k))

        e_stage1(0)
        for i in range(16):
            if i + 1 < 16:
                e_stage1(i + 1)
            e_stage2(i)
        P.barrier()
        A.release()

    if "F" in phases:
        A.mark()
        GU = 256
        NGU = 4
        NDS = 3
        hsb = [A.alloc([2, D], BF16) for _ in range(2)]
        hTe = [A.alloc([KC, CAP], BF16) for _ in range(2)]
        gub = [A.alloc([KC, GU], BF16) for _ in range(NGU)]
        wdb = [A.alloc([8, 512], BF16) for _ in range(NDS)]
        aT = [A.alloc([8, CAP], BF16) for _ in range(2)]
        sg = [A.alloc([CAP], F32) for _ in range(2)]
        yo = [A.alloc([512], F32) for _ in range(4)]
        wg_v = wg.rearrange("(e kc p) n -> e p kc n", p=128, kc=KC)
        wu_v = wu.rearrange("(e kc p) n -> e p kc n", p=128, kc=KC)
        wd_v = wd.rearrange("(e kc p) n -> e p kc n", p=128, kc=8)
        gu_items = [(e_, m2, w) for e_ in range(NEXP) for m2 in range(4) for w in (0, 1)]
        d_items = [(e_, cg) for e_ in range(NEXP) for cg in range(8)]
        gu_next = [0]
        d_next = [0]

        def ensure_gu(upto):
            while gu_next[0] <= min(upto, len(gu_items) - 1):
                k = gu_next[0]
                e_, m2, w = gu_items[k]
                s = k % NGU
                src = (wg_v if w == 0 else wu_v)[e_][:, :, m2 * GU:(m2 + 1) * GU]
                P.add("pool", lambda e, s=s, src=src: e.dma_start(out=gub[s], in_=src),
                      writes=[("gub", s)], dma_key=("gub", s))
                gu_next[0] += 1

        def ensure_d(upto):
            while d_next[0] <= min(upto, len(d_items) - 1):
                k = d_next[0]
                e_, cg = d_items[k]
                s = k % NDS
                src = wd_v[e_][:, :, cg * 512:(cg + 1) * 512]
                P.add("pool", lambda e, s=s, src=src: e.dma_start(out=wdb[s], in_=src),
                      writes=[("wdb", s)], dma_key=("wdb", s))
                d_next[0] += 1

        def load_hs(e_):
            s = e_ % 2
            P.add("sp", lambda e, e_=e_, s=s: e.dma_start(out=hsb[s][:, 0, :], in_=Hs[e_ * CAP:e_ * CAP + 128, :]),
                  reads=["Hs"], accs=[("hsb", s)], dma_key=("hsb", s, 0))
            P.add("sp", lambda e, e_=e_, s=s: e.dma_start(out=hsb[s][0:CAP - 128, 1, :],
                                                         in_=Hs[e_ * CAP + 128:(e_ + 1) * CAP, :]),
                  reads=["Hs"], accs=[("hsb", s)], dma_key=("hsb", s, 1))

        load_hs(0)
        gi = 0
        di = 0
        yi = 0
        for e_ in range(NEXP):
            s = e_ % 2
            if e_ + 1 < NEXP:
                load_hs(e_ + 1)
            for sbk in range(2):
                p0, pn = (0, 128) if sbk == 0 else (128, CAP - 128)
                for c in range(KC):
                    pb = 4 + (c // 4) % 2
                    P.add("pe", lambda e, c=c, pb=pb, sbk=sbk, s=s, pn=pn: e.transpose(
                        out=bank(pb, 512, BF16)[:, (c % 4) * 128:(c % 4) * 128 + pn],
                        in_=hsb[s][0:pn, sbk, c * 128:(c + 1) * 128], identity=ident_bf[0:pn, 0:pn]),
                        reads=[("hsb", s), "ident_bf"], writes=[("ps", pb)])
                    if c % 4 == 3:
                        c0 = c - 3
                        src = bank(pb, 512, BF16).rearrange("p (a b) -> p a b", b=128)[:, :, 0:pn]
                        if (c // 4) % 2:
                            P.add("dve", lambda e, c0=c0, src=src, p0=p0, pn=pn, s=s: e.tensor_copy(
                                out=hTe[s][:, c0:c0 + 4, p0:p0 + pn], in_=src),
                                reads=[("ps", pb)], accs=[("hTe", s)])
                        else:
                            P.add("act", lambda e, c0=c0, src=src, p0=p0, pn=pn, s=s: e.copy(
                                out=hTe[s][:, c0:c0 + 4, p0:p0 + pn], in_=src),
                                reads=[("ps", pb)], accs=[("hTe", s)])
            for m2 in range(4):
                ensure_gu(gi + NGU - 1)
                sgk = gi % NGU
                suk = (gi + 1) % NGU
                for ct in range(2):
                    m = m2 * 2 + ct
                    pu = m % 2
                    bg = pu * 2
                    bu = pu * 2 + 1
                    for kc in range(KC):
                        P.add("pe", lambda e, kc=kc, ct=ct, bg=bg, sgk=sgk, s=s: e.matmul(
                            bank(bg, CAP), lhsT=gub[sgk][:, kc, ct * 128:(ct + 1) * 128], rhs=hTe[s][:, kc, :],
                            start=(kc == 0), stop=(kc == KC - 1)),
                            reads=[("gub", sgk), ("hTe", s)], writes=[("ps", bg)])
                    for kc in range(KC):
                        P.add("pe", lambda e, kc=kc, ct=ct, bu=bu, suk=suk, s=s: e.matmul(
                            bank(bu, CAP), lhsT=gub[suk][:, kc, ct * 128:(ct + 1) * 128], rhs=hTe[s][:, kc, :],
                            start=(kc == 0), stop=(kc == KC - 1)),
                            reads=[("gub", suk), ("hTe", s)], writes=[("ps", bu)])
                    P.add("act", lambda e, bg=bg, pu=pu: e.activation(out=sg[pu], in_=bank(bg, CAP), func=AF.Silu),
                          reads=[("ps", bg)], writes=[("sg", pu)])
                    P.add("dve", lambda e, bu=bu, pu=pu, m=m, s=s: e.tensor_tensor(
                        out=aT[s][:, m, :], in0=bank(bu, CAP), in1=sg[pu], op=ALU.mult),
                        reads=[("ps", bu), ("sg", pu)], accs=[("aT", s)])
                gi += 2
            for cg in range(8):
                ensure_d(di + NDS - 1)
                sd = di % NDS
                di += 1
                for sbk in range(2):
                    p0, pn = (0, 128) if sbk == 0 else (128, CAP - 128)
                    b = 6 + yi % 2
                    ys_ = yi % 4
                    yi += 1
                    for kc in range(8):
                        P.add("pe", lambda e, kc=kc, p0=p0, pn=pn, b=b, sd=sd, s=s: e.matmul(
                            bank(b)[0:pn, :], lhsT=aT[s][:, kc, p0:p0 + pn], rhs=wdb[sd][:, kc, :],
                            start=(kc == 0), stop=(kc == 7)),
                            reads=[("aT", s), ("wdb", sd)], writes=[("ps", b)])
                    if yi % 2:
                        P.add("act", lambda e, b=b, ys_=ys_, pn=pn: e.copy(out=yo[ys_][0:pn, :], in_=bank(b)[0:pn, :]),
                              reads=[("ps", b)], writes=[("yo", ys_)])
                    else:
                        P.add("dve", lambda e, b=b, ys_=ys_, pn=pn: e.tensor_copy(out=yo[ys_][0:pn, :], in_=bank(b)[0:pn, :]),
                              reads=[("ps", b)], writes=[("yo", ys_)])
                    r0 = e_ * CAP + p0
                    P.add("sp", lambda e, r0=r0, cg=cg, ys_=ys_, pn=pn: e.dma_start(
                        out=Ys[r0:r0 + pn, cg * 512:(cg + 1) * 512], in_=yo[ys_][0:pn, :]),
                        reads=[("yo", ys_)], accs=["Ys"], dma_key=("yo", ys_))
        P.barrier()
        A.release()

    if "G" in phases:
        A.mark()
        xg = [A.alloc([D], F32) for _ in range(3)]
        y1 = [A.alloc([D], F32) for _ in range(3)]
        y2 = [A.alloc([D], F32) for _ in range(3)]
        for i in range(16):
            s = i % 3
            P.add("sp", lambda e, i=i, s=s: e.dma_start(out=xg[s], in_=x1_s[i * 128:(i + 1) * 128, :]),
                  reads=["x1_s"], writes=[("xg", s)], dma_key=("xg", s))
            if use_moe:
                for k, yb in ((0, y1), (1, y2)):
                    P.add("pool", lambda e, i=i, k=k, s=s, yb=yb: e.indirect_dma_start(
                        out=yb[s], out_offset=None, in_=Ys[:, :],
                        in_offset=bass.IndirectOffsetOnAxis(ap=gidx_i[:, i, k:k + 1], axis=0)),
                        reads=["Ys", "gidx_i"], writes=[("y", k, s)], dma_key=("yg", k, s))
                    P.add("dve", lambda e, i=i, k=k, s=s, yb=yb: e.scalar_tensor_tensor(
                        out=xg[s], in0=yb[s], scalar=gate_all[:, i, k:k + 1], in1=xg[s], op0=ALU.mult, op1=ALU.add),
                        reads=[("y", k, s), ("xg", s), "gate_all"], writes=[("xg", s)])
            P.add("sp", lambda e, i=i, s=s: e.dma_start(out=out[i * 128:(i + 1) * 128, :], in_=xg[s]),
                  reads=[("xg", s)], accs=["out"], dma_key=("og", s))
        A.release()
    P.barrier()
    P.emit()
    return nc, P


def make_consts():
    ident = np.eye(128, dtype=np.float32)
    ones = np.ones((128, 128), np.float32)
    ltri = np.triu(np.ones((128, 128), np.float32), k=1)
    kp = np.arange(128)[:, None]
    col = np.arange(640)[None, :]
    kt = col // 128
    q = col % 128
    krel = kt * 128 + kp
    qrel = 512 + q
    cq = qrel // 64
    ck = krel // 64
    valid = (cq - ck >= 0) & (cq - ck <= 8)
    maskc = np.where(valid, 0.0, NEG).astype(np.float32)
    rel_idx = np.clip(qrel - krel, -128, 128) + 128
    ebase = np.broadcast_to((np.arange(32, dtype=np.float32) * CAP)[None, :], (128, 32)).copy()
    return ident, ones, ltri, maskc, rel_idx, ebase


def make_in_maps(inputs, cores=range(8), with_moe=True):
    f = lambda a: np.ascontiguousarray(np.asarray(a, dtype=np.float32))
    x = f(inputs["x"])
    ident, ones, ltri, maskc, rel_idx, ebase = make_consts()
    w_in = f(inputs["w_in"][0])
    w_out = f(inputs["w_out"][0])
    g1_bc = np.ascontiguousarray(np.broadcast_to(f(inputs["norm1_gain"][0])[None, :], (128, D)))
    g2_bc = np.ascontiguousarray(np.broadcast_to(f(inputs["norm2_gain"][0])[None, :], (128, D)))
    qg_col = f(inputs["q_norm_gain"][0]).reshape(128, 1)
    kg_col = f(inputs["k_norm_gain"][0]).reshape(128, 1)
    pscale = np.ascontiguousarray(f(inputs["pool_scale"][0]).reshape(8, 128).T)
    gw = f(inputs["pool_group_w"][0])
    gw_l = np.ascontiguousarray(gw.reshape(4, 2, 128, 256).transpose(2, 0, 1, 3).reshape(128, 4 * 2 * 256))
    rb = f(inputs["rel_bias"][0])
    biasT = np.ascontiguousarray(rb[:, rel_idx])
    wrg = f(inputs["w_router_group"][0])
    wre = f(inputs["w_router_expert"][0])
    wr = np.concatenate([wrg, wre.transpose(1, 0, 2).reshape(D, 32)], axis=1)
    wr_l = np.ascontiguousarray(wr.reshape(KC, 128, 36).transpose(1, 0, 2).reshape(128, KC * 36))
    br = np.concatenate([f(inputs["b_router_group"][0]), f(inputs["b_router_expert"][0]).reshape(32)])
    br_bc = np.ascontiguousarray(np.broadcast_to(br[None, :], (128, 36)))
    if with_moe:
        wg = f(inputs["w_expert_gate"][0]).reshape(NEXP * D, 1024)
        wu = f(inputs["w_expert_up"][0]).reshape(NEXP * D, 1024)
        wd = f(inputs["w_expert_down"][0]).reshape(NEXP * 1024, D)
    maps = []
    for c in cores:
        b, half = c // 2, c % 2
        xh = np.zeros((T_EXT, D), np.float32)
        if half == 1:
            xh[:] = x[b, T_OWN - HALO:2 * T_OWN]
        else:
            xh[HALO:] = x[b, 0:T_OWN]
        t_abs = np.arange(T_OWN) + half * T_OWN
        invc = np.stack([1.0 / np.minimum(t_abs + 1, w) for w in (2, 4, 8, 16)]).astype(np.float32)
        invc_bc = np.ascontiguousarray(np.broadcast_to(invc.reshape(1, 4 * T_OWN), (128, 4 * T_OWN)))
        halo_col = np.full((128, 1), 0.0 if half == 1 else NEG, np.float32)
        m = dict(xh=xh, w_in=w_in, w_out=w_out, g1_bc=g1_bc, g2_bc=g2_bc, qg_col=qg_col, kg_col=kg_col,
                 pscale=pscale, gw=gw_l, invc=invc_bc, biasT=biasT, maskc=maskc, halo_col=halo_col, wr=wr_l,
                 br_bc=br_bc, ebase_bc=ebase, ident=ident, ones=ones, ltri=ltri)
        if with_moe:
            m.update(wg=wg, wu=wu, wd=wd)
        maps.append(m)
    return maps


def kernel(**inputs):
    nc, _ = build_program("ABCDEFG", dbg=False)
    maps = make_in_maps(inputs)
    res = run_bass_kernel_spmd(nc, maps, core_ids=list(range(8)))
    outs = [np.asarray(r["out"]) for r in res.results]
    y = np.stack(outs, 0).reshape(4, 2 * T_OWN, D).astype(np.float32)
    return y
```
